# Optimizing a Trainium2 kernel written in Bass

```python
import math
import jax, jax.numpy as jnp
from jax import lax
import numpy as np

D_MODEL = 2048
BATCH = 8
SEQ = 4096
DEPTH = 4

N_BRANCH = 4
BRANCH_W = D_MODEL // 4
RET_HEADS = 4
RET_DK = BRANCH_W // (2 * RET_HEADS)
RET_DV = BRANCH_W // RET_HEADS
RET_CHUNK = 128
ROPE_BASE = 10000.0
S5_GROUP = 16
S5_GROUPS = BRANCH_W // S5_GROUP
S5_STATE = 64
GLA_HEADS = 4
GLA_DK = BRANCH_W // (2 * GLA_HEADS)
GLA_DV = BRANCH_W // GLA_HEADS
GLA_RANK = 16
GLA_GATE_TEMP = 16.0
GLA_CHUNK = 16
POOL_WINDOWS = (2, 4, 8, 16)
POOL_GROUP = BRANCH_W // len(POOL_WINDOWS)
POOL_PAD = max(POOL_WINDOWS)
D_FF = 5504
CONV_W = 3
EPS = 1e-6

IN_SIZES = (
    RET_HEADS * RET_DK, RET_HEADS * RET_DK, RET_HEADS * RET_DV, BRANCH_W,
    BRANCH_W,
    GLA_HEADS * GLA_DK, GLA_HEADS * GLA_DK, GLA_HEADS * GLA_DV, BRANCH_W, GLA_RANK,
    BRANCH_W,
    N_BRANCH * D_MODEL,
)
N_IN = sum(IN_SIZES)

kernel_name = 'hybrid_gated_parallel_mixer'


def _split_points():
    pts, acc = [], 0
    for s in IN_SIZES[:-1]:
        acc += s
        pts.append(acc)
    return pts


def rmsnorm(x, g):
    xf = x.astype(jnp.float32)
    r = lax.rsqrt(jnp.mean(xf * xf, axis=-1, keepdims=True) + EPS)
    return (xf * r * g.astype(jnp.float32)).astype(x.dtype)


def head_norm(o):
    mu = jnp.mean(o, axis=-1, keepdims=True)
    var = jnp.mean(jnp.square(o - mu), axis=-1, keepdims=True)
    return (o - mu) * lax.rsqrt(var + EPS)


def rotary(x, positions):
    d = x.shape[-1]
    inv = ROPE_BASE ** (-jnp.arange(0, d, 2, dtype=jnp.float32) / d)
    ang = positions.astype(jnp.float32)[..., None] * inv
    cos = jnp.cos(ang)[:, :, None, :]
    sin = jnp.sin(ang)[:, :, None, :]
    x1, x2 = x[..., : d // 2], x[..., d // 2:]
    return jnp.concatenate([x1 * cos - x2 * sin, x1 * sin + x2 * cos], axis=-1)


def retention(q, k, v):
    B_, S_, H, dk = q.shape
    dv = v.shape[-1]
    C = RET_CHUNK
    n = S_ // C
    log_g = jnp.log(1.0 - 2.0 ** (-5.0 - jnp.arange(H, dtype=jnp.float32)))
    q = q.reshape(B_, n, C, H, dk)
    k = k.reshape(B_, n, C, H, dk)
    v = v.reshape(B_, n, C, H, dv)
    idx = jnp.arange(C, dtype=jnp.float32)
    rel = idx[:, None] - idx[None, :]
    causal = rel >= 0
    decay = jnp.where(causal[None], jnp.exp(jnp.where(causal, rel, 0.0)[None] * log_g[:, None, None]), 0.0)
    scores = jnp.einsum('bnthd,bnshd->bnhts', q, k) * decay
    intra = jnp.einsum('bnhts,bnshe->bnthe', scores, v)
    k_dec = k * jnp.exp((C - 1.0 - idx)[:, None] * log_g[None, :])[None, None, :, :, None]
    chunk_state = jnp.einsum('bnshd,bnshe->nbhde', k_dec, v)
    chunk_decay = jnp.exp(C * log_g)[None, :, None, None]

    def step(R, s):
        return chunk_decay * R + s, R

    _, R_prev = lax.scan(step, jnp.zeros((B_, H, dk, dv), jnp.float32), chunk_state)
    q_dec = q * jnp.exp((idx + 1.0)[:, None] * log_g[None, :])[None, None, :, :, None]
    cross = jnp.einsum('bnthd,nbhde->bnthe', q_dec, R_prev)
    return (intra + cross).reshape(B_, S_, H, dv)


def s5_branch(u, a_re, a_im, log_dt, b_re, b_im, c_re, c_im, d_skip, w_glu):
    B_, S_, _ = u.shape
    ug = u.reshape(B_, S_, S5_GROUPS, S5_GROUP)
    a_re = a_re.astype(jnp.float32)
    a_im = a_im.astype(jnp.float32)
    dt = jnp.exp(log_dt.astype(jnp.float32))[:, None]
    mag = jnp.exp(a_re * dt)
    abar_re = mag * jnp.cos(a_im * dt)
    abar_im = mag * jnp.sin(a_im * dt)
    den = a_re * a_re + a_im * a_im
    nr, ni = abar_re - 1.0, abar_im
    f_re = (nr * a_re + ni * a_im) / den
    f_im = (ni * a_re - nr * a_im) / den
    b_re = b_re.astype(jnp.float32)
    b_im = b_im.astype(jnp.float32)
    bb_re = f_re[..., None] * b_re - f_im[..., None] * b_im
    bb_im = f_re[..., None] * b_im + f_im[..., None] * b_re
    bu_re = jnp.einsum('bsgc,gpc->bsgp', ug, bb_re)
    bu_im = jnp.einsum('bsgc,gpc->bsgp', ug, bb_im)
    ar = jnp.broadcast_to(abar_re[None, None], bu_re.shape)
    ai = jnp.broadcast_to(abar_im[None, None], bu_re.shape)

    def combine(l, r):
        ar1, ai1, br1, bi1 = l
        ar2, ai2, br2, bi2 = r
        return (ar2 * ar1 - ai2 * ai1, ar2 * ai1 + ai2 * ar1,
                ar2 * br1 - ai2 * bi1 + br2, ar2 * bi1 + ai2 * br1 + bi2)

    _, _, xr, xi = lax.associative_scan(combine, (ar, ai, bu_re, bu_im), axis=1)
    y = jnp.einsum('bsgp,gcp->bsgc', xr, c_re) - jnp.einsum('bsgp,gcp->bsgc', xi, c_im)
    y = y + d_skip.reshape(S5_GROUPS, S5_GROUP) * ug
    y = jax.nn.gelu(y.reshape(B_, S_, BRANCH_W))
    return y * jax.nn.sigmoid(y @ w_glu)


def gla(q, k, v, log_a):
    B_, S_, H, dk = q.shape
    dv = v.shape[-1]
    C = GLA_CHUNK
    n = S_ // C
    q = q.reshape(B_, n, C, H, dk)
    k = k.reshape(B_, n, C, H, dk)
    v = v.reshape(B_, n, C, H, dv)
    b = jnp.cumsum(log_a.reshape(B_, n, C, H, dk), axis=2)
    idx = jnp.arange(C)
    mask = (idx[:, None] >= idx[None, :])[None, None, :, :, None, None]
    diff = b[:, :, :, None] - b[:, :, None, :]
    w = jnp.where(mask, jnp.exp(jnp.where(mask, diff, 0.0)), 0.0)
    scores = jnp.einsum('bnthd,bnshd,bntshd->bnhts', q, k, w)
    intra = jnp.einsum('bnhts,bnshe->bnthe', scores, v)
    b_last = b[:, :, -1]
    k_dec = k * jnp.exp(b_last[:, :, None] - b)
    chunk_state = jnp.einsum('bnshd,bnshe->nbhde', k_dec, v)
    chunk_decay = jnp.moveaxis(jnp.exp(b_last), 1, 0)

    def step(Sst, inp):
        s_in, dec = inp
        return dec[..., None] * Sst + s_in, Sst

    _, S_prev = lax.scan(step, jnp.zeros((B_, H, dk, dv), jnp.float32), (chunk_state, chunk_decay))
    cross = jnp.einsum('bnthd,nbhde->bnthe', q * jnp.exp(b), S_prev)
    return (intra + cross).reshape(B_, S_, H, dv)


def pool_branch(p, pool_w, pool_scale):
    B_, S_, _ = p.shape
    G = len(POOL_WINDOWS)
    pg = p.reshape(B_, S_, G, POOL_GROUP)
    cs = jnp.cumsum(pg, axis=1)
    cs_pad = jnp.concatenate([jnp.zeros((B_, POOL_PAD, G, POOL_GROUP), cs.dtype), cs], axis=1)
    pos1 = jnp.arange(S_, dtype=jnp.float32) + 1.0
    means = []
    for g, win in enumerate(POOL_WINDOWS):
        prev = cs_pad[:, POOL_PAD - win: POOL_PAD - win + S_, g]
        cnt = jnp.minimum(pos1, float(win))[None, :, None]
        means.append((cs[:, :, g] - prev) / cnt)
    mixed = jnp.stack(means, axis=2) - pg
    out = jnp.einsum('bsgc,gcd->bsgd', mixed, pool_w).reshape(B_, S_, BRANCH_W)
    return out * pool_scale


def hybrid_mixer(h, positions, w_in, s5_a_re, s5_a_im, s5_log_dt, s5_b_re, s5_b_im, s5_c_re, s5_c_im,
                 s5_d, s5_w_glu, gla_w_gate, gla_b_gate, pool_w, pool_scale, w_branch, w_out):
    B_, S_, _ = h.shape
    proj = (h @ w_in).astype(jnp.float32)
    rq, rk, rv, rg, su, gq, gk, gv, gg, gr, pu, gate_logits = jnp.split(proj, _split_points(), axis=-1)
    rq = rotary(rq.reshape(B_, S_, RET_HEADS, RET_DK), positions)
    rk = rotary(rk.reshape(B_, S_, RET_HEADS, RET_DK), positions) * (RET_DK ** -0.5)
    ra = retention(rq, rk, rv.reshape(B_, S_, RET_HEADS, RET_DV))
    ya = head_norm(ra).reshape(B_, S_, BRANCH_W) * jax.nn.silu(rg)
    yb = s5_branch(su, s5_a_re, s5_a_im, s5_log_dt, s5_b_re, s5_b_im, s5_c_re, s5_c_im, s5_d, s5_w_glu)
    log_a = jax.nn.log_sigmoid(gr @ gla_w_gate + gla_b_gate) / GLA_GATE_TEMP
    gc = gla(gq.reshape(B_, S_, GLA_HEADS, GLA_DK) * (GLA_DK ** -0.5),
             gk.reshape(B_, S_, GLA_HEADS, GLA_DK),
             gv.reshape(B_, S_, GLA_HEADS, GLA_DV),
             log_a.reshape(B_, S_, GLA_HEADS, GLA_DK))
    yc = head_norm(gc).reshape(B_, S_, BRANCH_W) * jax.nn.silu(gg)
    yd = pool_branch(pu, pool_w, pool_scale)
    branches = jnp.stack([ya, yb, yc, yd], axis=2)
    up = jnp.einsum('bsnc,ncd->bsnd', branches, w_branch)
    gates = jax.nn.sigmoid(gate_logits.reshape(B_, S_, N_BRANCH, D_MODEL))
    merged = jnp.sum(gates * up, axis=2)
    return merged @ w_out


def conv_ffn(h, w_up, conv_w, conv_b, w_down):
    S_ = h.shape[1]
    u = h @ w_up
    up = jnp.pad(u, ((0, 0), (CONV_W - 1, 0), (0, 0)))
    uc = conv_b + up[:, 0:S_] * conv_w[0] + up[:, 1:S_ + 1] * conv_w[1] + up[:, 2:S_ + 2] * conv_w[2]
    a, v = jnp.split(uc, 2, axis=-1)
    return (jax.nn.silu(a) * v) @ w_down


def setup_inputs(seed: int = 0) -> dict:
    key = jax.random.key(seed)
    ks = jax.random.split(key, 32)
    f32 = jnp.float32
    L = DEPTH

    def nrm(k, shape, scale):
        return jax.random.normal(k, shape, f32) * scale

    x = nrm(ks[0], (BATCH, SEQ, D_MODEL), 1.0)
    offsets = jax.random.randint(ks[1], (BATCH, 1), 0, 1024, dtype=jnp.int32)
    positions = offsets + jnp.arange(SEQ, dtype=jnp.int32)[None, :]
    norm_mix_g = 1.0 + nrm(ks[2], (L, D_MODEL), 0.02)
    w_in = nrm(ks[3], (L, D_MODEL, N_IN), D_MODEL ** -0.5)
    n_idx = jnp.arange(S5_STATE, dtype=f32)
    s5_a_re = -0.5 + nrm(ks[4], (L, S5_GROUPS, S5_STATE), 0.01)
    s5_a_im = math.pi * n_idx + nrm(ks[5], (L, S5_GROUPS, S5_STATE), 0.01)
    s5_log_dt = jax.random.uniform(ks[6], (L, S5_GROUPS), f32, math.log(1e-3), math.log(1e-1))
    s5_b_re = nrm(ks[7], (L, S5_GROUPS, S5_STATE, S5_GROUP), (2.0 * S5_GROUP) ** -0.5)
    s5_b_im = nrm(ks[8], (L, S5_GROUPS, S5_STATE, S5_GROUP), (2.0 * S5_GROUP) ** -0.5)
    s5_c_re = nrm(ks[9], (L, S5_GROUPS, S5_GROUP, S5_STATE), 2.0 ** -0.5)
    s5_c_im = nrm(ks[10], (L, S5_GROUPS, S5_GROUP, S5_STATE), 2.0 ** -0.5)
    s5_d = nrm(ks[11], (L, BRANCH_W), 1.0)
    s5_w_glu = nrm(ks[12], (L, BRANCH_W, BRANCH_W), BRANCH_W ** -0.5)
    gla_w_gate = nrm(ks[13], (L, GLA_RANK, GLA_HEADS * GLA_DK), GLA_RANK ** -0.5)
    gla_b_gate = nrm(ks[14], (L, GLA_HEADS * GLA_DK), 0.01)
    pool_w = nrm(ks[15], (L, len(POOL_WINDOWS), POOL_GROUP, POOL_GROUP), POOL_GROUP ** -0.5)
    pool_scale = 1.0 + nrm(ks[16], (L, BRANCH_W), 0.1)
    w_branch = nrm(ks[17], (L, N_BRANCH, BRANCH_W, D_MODEL), BRANCH_W ** -0.5)
    w_out = nrm(ks[18], (L, D_MODEL, D_MODEL), D_MODEL ** -0.5)
    norm_ffn_g = 1.0 + nrm(ks[19], (L, D_MODEL), 0.02)
    w_up = nrm(ks[20], (L, D_MODEL, 2 * D_FF), D_MODEL ** -0.5)
    conv_w = nrm(ks[21], (L, CONV_W, 2 * D_FF), CONV_W ** -0.5)
    conv_b = nrm(ks[22], (L, 2 * D_FF), 0.01)
    w_down = nrm(ks[23], (L, D_FF, D_MODEL), D_FF ** -0.5)
    final_g = 1.0 + nrm(ks[24], (D_MODEL,), 0.02)
    return {'x': x, 'positions': positions, 'norm_mix_g': norm_mix_g, 'w_in': w_in,
            's5_a_re': s5_a_re, 's5_a_im': s5_a_im, 's5_log_dt': s5_log_dt,
            's5_b_re': s5_b_re, 's5_b_im': s5_b_im, 's5_c_re': s5_c_re, 's5_c_im': s5_c_im,
            's5_d': s5_d, 's5_w_glu': s5_w_glu, 'gla_w_gate': gla_w_gate, 'gla_b_gate': gla_b_gate,
            'pool_w': pool_w, 'pool_scale': pool_scale, 'w_branch': w_branch, 'w_out': w_out,
            'norm_ffn_g': norm_ffn_g, 'w_up': w_up, 'conv_w': conv_w, 'conv_b': conv_b,
            'w_down': w_down, 'final_g': final_g}


def reference(x, positions, norm_mix_g, w_in, s5_a_re, s5_a_im, s5_log_dt, s5_b_re, s5_b_im,
              s5_c_re, s5_c_im, s5_d, s5_w_glu, gla_w_gate, gla_b_gate, pool_w, pool_scale,
              w_branch, w_out, norm_ffn_g, w_up, conv_w, conv_b, w_down, final_g):
    h = x
    for l in range(DEPTH):
        hn = rmsnorm(h, norm_mix_g[l])
        mix = hybrid_mixer(hn, positions, w_in[l], s5_a_re[l], s5_a_im[l], s5_log_dt[l],
                           s5_b_re[l], s5_b_im[l], s5_c_re[l], s5_c_im[l], s5_d[l], s5_w_glu[l],
                           gla_w_gate[l], gla_b_gate[l], pool_w[l], pool_scale[l], w_branch[l], w_out[l])
        h = h + mix.astype(h.dtype)
        hn = rmsnorm(h, norm_ffn_g[l])
        h = h + conv_ffn(hn, w_up[l], conv_w[l], conv_b[l], w_down[l]).astype(h.dtype)
    return rmsnorm(h, final_g)
```

```python
import math
import contextlib
import numpy as np
import concourse.bass as bass
import concourse.mybir as mybir
from concourse.bass_utils import run_bass_kernel_spmd

F32 = mybir.dt.float32
BF16 = mybir.dt.bfloat16
I32 = mybir.dt.int32
AF = mybir.ActivationFunctionType
ALU = mybir.AluOpType

D = 2048
DC = 16
T = 512
DFF = 5504
FC = 43
NIN = 12304
EPS = 1e-6
TWO_PI = 2.0 * math.pi
C1 = 6.28125
C2 = TWO_PI - C1

SAME_ENGINE_SYNC = True
NSLOT = 7
LOOKAHEAD = 3


def piece_names():
    n = []
    n += ["RQ0", "RQS0", "RQ1", "RQS1", "RK0", "RKS0", "RK1", "RKS1"]
    n += ["RV0", "RV1", "RV2", "RV3"]
    n += ["RG0", "RG1", "RG2", "RG3"]
    n += ["GR", "GQ0", "GQ1", "GK0", "GK1"]
    n += ["GV0", "GV1", "GV2", "GV3"]
    n += ["GG0", "GG1", "GG2", "GG3"]
    n += ["PU0", "PU1", "PU2", "PU3", "POOLW"]
    n += ["SU0", "SU1", "SU2", "SU3", "CRE", "CIM", "WGLU"]
    for dc in range(DC):
        n += [f"GATE0_{dc}", f"GATE2_{dc}", f"GATE3_{dc}", f"WBRA{dc}"]
    for q in range(8):
        n += [f"WBRB{q}", f"GATE1_{2 * q}", f"GATE1_{2 * q + 1}"]
    n += [f"WOUT{dc}" for dc in range(DC)]
    for fc in range(FC):
        n += [f"UPA{fc}", f"UPV{fc}"]
    for dc in range(DC):
        n += [f"WDN{dc}_{q}" for q in range(3)]
    return n


DIRECT = ("CRE", "CIM", "WGLU")

PIECES = piece_names()
NP_ = len(PIECES)
PIDX = {nm: i for i, nm in enumerate(PIECES)}


def _lhsT_piece(W, cols):
    K = W.shape[0]
    kc = K // 128
    sub = W[:, cols]
    out = np.zeros((128, 16, 128), np.float32)
    out[:, :kc, :sub.shape[1]] = sub.reshape(kc, 128, -1).transpose(1, 0, 2)
    return out.reshape(128, 2048)


def build_layer_pieces(inp, l):
    w_in = inp["w_in"][l]
    arr = np.zeros((NP_, 128, 2048), np.float32)
    ar = np.arange(128)

    def put(nm, a):
        arr[PIDX[nm]] = a

    hl, half, j = ar // 64, (ar % 64) // 32, ar % 32
    for c in range(2):
        base = (2 * c + hl) * 64
        same = base + half * 32 + j
        swap = base + (1 - half) * 32 + j
        put(f"RQ{c}", _lhsT_piece(w_in, 0 + same))
        put(f"RQS{c}", _lhsT_piece(w_in, 0 + swap))
        put(f"RK{c}", _lhsT_piece(w_in, 256 + same))
        put(f"RKS{c}", _lhsT_piece(w_in, 256 + swap))
    def vpieces(prefix, off):
        Wv = w_in[:, off:off + 512]
        r = Wv.reshape(16, 128, 512).transpose(1, 0, 2)
        for q in range(4):
            put(f"{prefix}{q}", r[:, 4 * q:4 * q + 4, :].reshape(128, 2048))
    vpieces("RV", 512)
    for c in range(4):
        put(f"RG{c}", _lhsT_piece(w_in, 1024 + c * 128 + ar))
        put(f"SU{c}", _lhsT_piece(w_in, 1536 + c * 128 + ar))
        put(f"GG{c}", _lhsT_piece(w_in, 3072 + c * 128 + ar))
        put(f"PU{c}", _lhsT_piece(w_in, 3600 + c * 128 + ar))
    for c in range(2):
        put(f"GQ{c}", _lhsT_piece(w_in, 2048 + c * 128 + ar))
        put(f"GK{c}", _lhsT_piece(w_in, 2304 + c * 128 + ar))
    vpieces("GV", 2560)
    put("GR", _lhsT_piece(w_in, 3584 + np.arange(16)))
    for nm, Cm in (("CRE", inp["s5_c_re"][l]), ("CIM", inp["s5_c_im"][l])):
        a = np.zeros((128, 16, 128), np.float32)
        for jj in range(16):
            for gi in range(2):
                g = 2 * jj + gi
                gl = g % 8
                a[gi * 64:(gi + 1) * 64, jj, gl * 16:(gl + 1) * 16] = Cm[g].T
        put(nm, a.reshape(128, 2048))
    wg = inp["s5_w_glu"][l]
    a = np.zeros((128, 16, 128), np.float32)
    for oc in range(4):
        for kc in range(4):
            a[:, oc * 4 + kc, :] = wg[kc * 128:(kc + 1) * 128, oc * 128:(oc + 1) * 128]
    put("WGLU", a.reshape(128, 2048))
    a = np.zeros((128, 16, 128), np.float32)
    for g in range(4):
        a[:, g, :] = inp["pool_w"][l][g]
    put("POOLW", a.reshape(128, 2048))
    wb = inp["w_branch"][l]
    for dc in range(DC):
        for k in range(4):
            put(f"GATE{k}_{dc}", _lhsT_piece(w_in, 4112 + k * D + dc * 128 + ar))
        a = np.zeros((128, 16, 128), np.float32)
        for ki_, k in enumerate((0, 2, 3)):
            for kc in range(4):
                a[:, ki_ * 4 + kc, :] = wb[k, kc * 128:(kc + 1) * 128, dc * 128:(dc + 1) * 128]
        put(f"WBRA{dc}", a.reshape(128, 2048))
        if dc % 2 == 0:
            a = np.zeros((128, 16, 128), np.float32)
            for dl in range(2):
                for kc in range(4):
                    a[:, dl * 4 + kc, :] = wb[1, kc * 128:(kc + 1) * 128, (dc + dl) * 128:(dc + dl + 1) * 128]
            put(f"WBRB{dc // 2}", a.reshape(128, 2048))
        put(f"WOUT{dc}", _lhsT_piece(inp["w_out"][l], dc * 128 + ar))
    w_up = inp["w_up"][l]
    for fc in range(FC):
        put(f"UPA{fc}", _lhsT_piece(w_up, fc * 128 + ar))
        put(f"UPV{fc}", _lhsT_piece(w_up, DFF + fc * 128 + ar))
    wd = inp["w_down"][l]
    for dc in range(DC):
        for q in range(3):
            k0 = q * 16 * 128
            k1 = min(DFF, k0 + 2048)
            put(f"WDN{dc}_{q}", _lhsT_piece(wd[k0:k1], dc * 128 + ar))
    return arr


def par_layout():
    o = {}
    off = 0
    def add(nm, n):
        nonlocal off
        o[nm] = (off, n)
        off += n
    add("g_mix", 16); add("g_ffn", 16)
    add("cw0", 86); add("cw1", 86); add("cw2", 86); add("cb", 86)
    add("pool_scale", 4); add("s5_d", 4)
    add("a_re", 16); add("a_im", 16); add("logdt", 16)
    add("wgate", 256)
    return o, off


PAR, NPAR = par_layout()


def build_layer_params(inp, l):
    a = np.zeros((128, NPAR), np.float32)
    def put(nm, v):
        o, n = PAR[nm]
        a[:v.shape[0], o:o + n] = v
    put("g_mix", inp["norm_mix_g"][l].reshape(16, 128).T)
    put("g_ffn", inp["norm_ffn_g"][l].reshape(16, 128).T)
    cw = inp["conv_w"][l]
    put("cw0", cw[0].reshape(86, 128).T); put("cw1", cw[1].reshape(86, 128).T); put("cw2", cw[2].reshape(86, 128).T)
    put("cb", inp["conv_b"][l].reshape(86, 128).T)
    put("pool_scale", inp["pool_scale"][l].reshape(4, 128).T)
    put("s5_d", inp["s5_d"][l].reshape(4, 128).T)
    put("a_re", inp["s5_a_re"][l].reshape(16, 128).T)
    put("a_im", inp["s5_a_im"][l].reshape(16, 128).T)
    put("logdt", np.repeat(inp["s5_log_dt"][l], 64).reshape(16, 128).T)
    wg = np.concatenate([inp["gla_w_gate"][l], inp["gla_b_gate"][l][None, :]], axis=0)
    put("wgate", wg)
    return a


def build_bt(inp, l):
    out = np.zeros((2, 128, 16, 128), np.float32)
    for k, Bm in enumerate((inp["s5_b_re"][l], inp["s5_b_im"][l])):
        for jj in range(16):
            for gi in range(2):
                g = 2 * jj + gi
                gl = g % 8
                out[k, gl * 16:(gl + 1) * 16, jj, gi * 64:(gi + 1) * 64] = Bm[g].T
    return out.reshape(2, 128, 2048)


def build_consts():
    c = {}
    ar = np.arange(128)
    c["ident"] = np.eye(128, dtype=np.float32)
    c["ones"] = np.ones((128, 128), np.float32)
    s, t = np.meshgrid(ar, ar, indexing="ij")
    c["umask"] = (s <= t).astype(np.float32)
    j = ar % 32
    c["invf"] = (np.float32(10000.0) ** (-(2 * j).astype(np.float32) / np.float32(64))).astype(np.float32)[:, None]
    sgn = np.where((ar % 64) < 32, -1.0, 1.0)
    tau = np.arange(T) % 128
    dec = np.zeros((8, 128, T), np.float32)
    eret = np.zeros((128, 2), np.float32)
    for cc in range(2):
        h = 2 * cc + ar // 64
        lg = np.log(1.0 - 2.0 ** (-5.0 - h.astype(np.float64)))
        dq = np.exp(lg[:, None] * (tau[None, :] + 1.0))
        dk = 0.125 * np.exp(-lg[:, None] * (tau[None, :] + 1.0))
        dec[2 * cc + 0] = dq
        dec[2 * cc + 1] = dq * sgn[:, None]
        dec[4 + 2 * cc + 0] = dk
        dec[4 + 2 * cc + 1] = dk * sgn[:, None]
        eret[:, cc] = np.exp(128.0 * lg)
    c["dec"] = dec
    c["eret"] = eret
    ic = np.zeros((128, 4, 16), np.float32)
    for g, w in enumerate((2, 4, 8, 16)):
        ic[:, g, :] = 1.0 / np.minimum(np.arange(16) + 1.0, float(w))
    c["invcnt"] = ic
    c["iota"] = np.broadcast_to(np.arange(T, dtype=np.float32)[None, :], (128, T)).copy()
    return c


class Op:
    __slots__ = ("eng", "fn", "deps", "stream", "ordinal", "needs_inc")


class Prog:
    ENGS = ("pe", "act", "dve", "pool", "sp")

    def __init__(self):
        self.ops = {e: [] for e in self.ENGS}
        self.last_w = {}
        self.readers = {}
        self.last_dma = {}
        self.epoch = 0
        self.streams = []
        self._sset = set()

    def op(self, eng, fn, reads=(), writes=(), dma_sem=None):
        o = Op()
        o.eng = eng
        o.fn = fn
        o.needs_inc = False
        o.ordinal = 0
        deps = set()
        for k in reads:
            w = self.last_w.get(k)
            if w is not None:
                deps.add(w)
        for k in writes:
            w = self.last_w.get(k)
            if w is not None:
                deps.add(w)
            for r in self.readers.get(k, ()):
                deps.add(r)
        if dma_sem is not None:
            o.stream = ("dma", dma_sem)
            prev = self.last_dma.get(dma_sem)
            if prev is not None:
                deps.add(prev)
            self.last_dma[dma_sem] = o
        else:
            o.stream = (eng, self.epoch)
        if o.stream not in self._sset:
            self._sset.add(o.stream)
            self.streams.append(o.stream)
        deps.discard(o)
        o.deps = deps
        for k in writes:
            self.last_w[k] = o
            self.readers[k] = []
        for k in reads:
            self.readers.setdefault(k, []).append(o)
        self.ops[eng].append(o)
        return o

    @staticmethod
    def need_wait(o, d):
        if d.stream[0] == "dma":
            return True
        if d.eng == o.eng:
            if o.eng == "pe":
                return False
            if o.eng == "pool":
                return True
            return SAME_ENGINE_SYNC
        return True

    def finalize(self):
        for e in self.ENGS:
            for o in self.ops[e]:
                for d in o.deps:
                    if self.need_wait(o, d):
                        d.needs_inc = True
        cnt = {}
        for e in self.ENGS:
            for o in self.ops[e]:
                if o.stream[0] == "dma":
                    cnt[o.stream] = cnt.get(o.stream, 0) + 16
                    o.ordinal = cnt[o.stream]
                    o.needs_inc = True
                elif o.needs_inc:
                    cnt[o.stream] = cnt.get(o.stream, 0) + 1
                    o.ordinal = cnt[o.stream]
        self.maxcount = cnt

    def emit(self, eng_name, e, sems):
        seen = {}
        nwait = 0
        for o in self.ops[eng_name]:
            need = {}
            for d in o.deps:
                if self.need_wait(o, d):
                    if d.ordinal > need.get(d.stream, 0):
                        need[d.stream] = d.ordinal
            for st, val in need.items():
                if seen.get(st, 0) >= val:
                    continue
                e.wait_ge(sems[st], val)
                seen[st] = val
                nwait += 1
            ins = o.fn(e)
            if o.needs_inc:
                ins.then_inc(sems[o.stream], 16 if o.stream[0] == "dma" else 1)
        return nwait


def build_program(S, depth, flags=None):
    flags = flags or {}
    NT = S // T
    nc = bass.Bass("TRN2", target_bir_lowering=False, dynamic_dma_scratch_size=1024)
    P = Prog()
    es = contextlib.ExitStack()

    def dram_in(name, shape, dt=F32):
        return nc.dram_tensor(name, list(shape), dt, kind="ExternalInput").ap()

    xT = dram_in("xT", [D, S])
    pos = dram_in("pos", [1, S], I32)
    wf = [dram_in(f"wf{l}", [NP_, 128, 2048]) for l in range(depth)]
    par_d = [dram_in(f"par{l}", [128, NPAR]) for l in range(depth)]
    bt_d = [dram_in(f"bt{l}", [2, 128, 2048]) for l in range(depth)]
    fing_d = dram_in("fing", [128, 16])
    cst = build_consts()
    cst_d = {k: dram_in("c_" + k, v.shape) for k, v in cst.items()}
    outT = nc.dram_tensor("outT", [D, S], F32, kind="ExternalOutput").ap()
    wb = [nc.dram_tensor(f"wb{l}", [NP_, 128, 2048], BF16, kind="Internal").ap() for l in range(depth)]
    hscr = nc.dram_tensor("hscr", [D, S], F32, kind="Internal").ap()
    rot_d = nc.dram_tensor("rot", [NT, 8, 128, T], F32, kind="Internal").ap()
    rot5_d = nc.dram_tensor("rot5", [16, 2, 128, T], F32, kind="Internal").ap()
    rs_d = nc.dram_tensor("rsd", [NT, 128, T], F32, kind="Internal").ap()

    def sb(name, shape, dt):
        return es.enter_context(nc.sbuf_tensor(name, list(shape), dt))

    ring = sb("ring", [128, NSLOT, 2048], BF16)
    NB = 62
    NF = 16
    Bt = sb("Bt", [128, NB, T], BF16)
    Ft = sb("Ft", [128, NF, T], F32)
    hT = sb("hT", [128, DC, T], F32)
    hn = sb("hn", [128, DC, T], BF16)
    par = sb("par", [128, NPAR], F32)
    fing = sb("fing_s", [128, 16], F32)
    ident = sb("ident", [128, 128], F32)
    ones_f = sb("ones_f", [128, 128], F32)
    ones_b = sb("ones_b", [128, 128], BF16)
    umask_f = sb("umask_f", [128, 128], F32)
    mask_b = sb("mask_b", [128, T], BF16)
    invf = sb("invf", [128, 1], F32)
    eret = sb("eret", [128, 2], F32)
    iota = sb("iota", [128, T], F32)
    posi = sb("posi", [128, T], I32)
    ki = sb("ki", [128, T], I32)
    halo = sb("halo", [128, 86, 2], F32)
    ubuf = sb("ubuf", [128, 2, T + 2], F32)
    pbuf = sb("pbuf", [128, 4, 16 + T], F32)
    Rst = sb("Rst", [128, 4, 256], F32)
    Rb = sb("Rb", [128, 8, 256], BF16)
    ktok = sb("ktok", [128, 2, 128], BF16)
    vtok = sb("vtok", [128, 4, T], BF16)
    graug = sb("graug", [32, T], F32)
    nla = sb("nla", [128, 256], F32)
    s5m = sb("s5m", [128, 16, 26], F32)
    s5z = sb("s5z", [128, 16, 2], F32)
    s5zt = sb("s5zt", [128, 16, 4], F32)
    s5zl = sb("s5zl", [128, 16, 2], F32)
    bbT = sb("bbT", [128, 2, 16, 128], BF16)
    dgd = sb("dgd", [128, 4, 128], BF16)
    dgtmp = sb("dgtmp", [128, 128], F32)
    s5mi = sb("s5mi", [128, 16], I32)
    onec = sb("onec", [128, 1], F32)
    invcnt = sb("invcnt", [128, 4, 16], F32)
    ps = [es.enter_context(nc.psum_tensor(f"ps{i}", [128, T], F32)) for i in range(8)]

    bank_ctr = [0]

    def bank():
        b = bank_ctr[0] % 7
        bank_ctr[0] += 1
        return b

    def B(i):
        return Bt[:, i, :]

    def Fv(i):
        return Ft[:, i, :]

    def dma(out_ap, in_ap, reads, writes, sem, eng="sp", **kw):
        return P.op(eng, lambda e: e.dma_start(out=out_ap, in_=in_ap, **kw), reads, writes, dma_sem=sem)

    def dve(fn, reads, writes):
        return P.op("dve", fn, reads, writes)

    def act(fn, reads, writes):
        return P.op("act", fn, reads, writes)

    def pe(fn, reads, writes):
        return P.op("pe", fn, reads, writes)

    def mm_group(out_ap, pairs, reads, writes):
        def fn(e):
            n = len(pairs)
            ins = None
            for i, (l, r) in enumerate(pairs):
                ins = e.matmul(out_ap, lhsT=l, rhs=r, start=(i == 0), stop=(i == n - 1))
            return ins
        return pe(fn, reads, writes)

    def activation(out_ap, in_ap, func, reads, writes, scale=1.0, bias=None):
        def fn(e):
            if bias is None:
                return e.activation(out=out_ap, in_=in_ap, func=func, scale=scale)
            return e.activation(out=out_ap, in_=in_ap, func=func, scale=scale, bias=bias)
        return act(fn, reads, writes)

    def tt(out_ap, a, b, op, reads, writes, eng="dve"):
        return P.op(eng, lambda e: e.tensor_tensor(out=out_ap, in0=a, in1=b, op=op), reads, writes)

    def ts(out_ap, a, s1, s2, op0, op1, reads, writes, eng="dve"):
        if s2 is None:
            return P.op(eng, lambda e: e.tensor_scalar(out=out_ap, in0=a, scalar1=s1, scalar2=None, op0=op0), reads, writes)
        return P.op(eng, lambda e: e.tensor_scalar(out=out_ap, in0=a, scalar1=s1, scalar2=s2, op0=op0, op1=op1), reads, writes)

    def stt(out_ap, a, s, b, op0, op1, reads, writes):
        return dve(lambda e: e.scalar_tensor_tensor(out=out_ap, in0=a, scalar=s, in1=b, op0=op0, op1=op1), reads, writes)

    def copy(out_ap, in_ap, reads, writes, eng="dve"):
        return P.op(eng, lambda e: e.tensor_copy(out=out_ap, in_=in_ap), reads, writes)

    def memset(ap, val, writes, eng="dve"):
        return P.op(eng, lambda e: e.memset(ap, val), (), writes)

    seq = []
    for l in range(depth):
        for t in range(NT):
            for i in range(NP_):
                if PIECES[i] not in DIRECT:
                    seq.append((l, i))
    NRING = NP_ - len(DIRECT)
    wstate = {"next_load": 0, "next_use": 0}

    def issue_load():
        k = wstate["next_load"]
        if k >= len(seq):
            return
        l, i = seq[k]
        slot = k % NSLOT
        dma(ring[:, slot, :], wb[l][i], [("wb", l, i)], [("ring", slot)], f"ring{slot}")
        wstate["next_load"] = k + 1

    def nextw(name):
        k = wstate["next_use"]
        l, i = seq[k]
        assert PIECES[i] == name, (PIECES[i], name)
        while wstate["next_load"] <= min(k + LOOKAHEAD, len(seq) - 1):
            issue_load()
        wstate["next_use"] = k + 1
        slot = k % NSLOT
        if l + 1 < depth:
            tt_ = k // NRING - l * NT
            if NT > 1:
                target = min(NP_, ((tt_ * NP_ + i + 1) * NP_) // ((NT - 1) * NP_) + 1)
            else:
                target = NP_
            while cast_next[l + 1] < target:
                issue_cast(l + 1, cast_next[l + 1])
                cast_next[l + 1] += 1
        return ring[:, slot, :], ("ring", slot)

    def lhs(wap, kc, m=128):
        return wap[:, kc * 128: kc * 128 + m]

    def sincos(ang, sin_out, cos_out, tmp, tmpi, keys_in, k_sin, k_cos, k_tmp, k_tmpi, shape_slice=None):
        t0, t1 = tmp
        ts(t0, ang, 1.0 / TWO_PI, None, ALU.mult, None, keys_in, [k_tmp[0]])
        copy(tmpi, t0, [k_tmp[0]], [k_tmpi])
        copy(t0, tmpi, [k_tmpi], [k_tmp[0]])
        stt(t1, t0, -C1, ang, ALU.mult, ALU.add, [k_tmp[0]] + list(keys_in), [k_tmp[1]])
        stt(t1, t0, -C2, t1, ALU.mult, ALU.add, [k_tmp[0], k_tmp[1]], [k_tmp[1]])
        ts(t0, t1, math.pi, TWO_PI, ALU.is_gt, ALU.mult, [k_tmp[1]], [k_tmp[0]])
        tt(t1, t1, t0, ALU.subtract, [k_tmp[0], k_tmp[1]], [k_tmp[1]])
        ts(t0, t1, -math.pi, TWO_PI, ALU.is_lt, ALU.mult, [k_tmp[1]], [k_tmp[0]])
        tt(t1, t1, t0, ALU.add, [k_tmp[0], k_tmp[1]], [k_tmp[1]])
        ts(t0, t1, math.pi / 2, None, ALU.add, None, [k_tmp[1]], [k_tmp[0]])
        ts(cos_out, t0, math.pi, TWO_PI, ALU.is_gt, ALU.mult, [k_tmp[0]], [k_cos])
        tt(t0, t0, cos_out, ALU.subtract, [k_tmp[0], k_cos], [k_tmp[0]])
        LIM = 3.1415925
        ts(t0, t0, -LIM, LIM, ALU.max, ALU.min, [k_tmp[0]], [k_tmp[0]])
        ts(t1, t1, -LIM, LIM, ALU.max, ALU.min, [k_tmp[1]], [k_tmp[1]])
        activation(sin_out, t1, AF.Sin, [k_tmp[1]], [k_sin])
        activation(cos_out, t0, AF.Sin, [k_tmp[0]], [k_cos])

    cast_state = {"k": 0}

    def issue_cast(l, i):
        k = cast_state["k"]
        dma(wb[l][i], wf[l][i], [], [("wb", l, i)], f"cast{k % 4}", eng="pool", max_dma_last_dim=8192)
        cast_state["k"] = k + 1

    cast_next = [0] * (depth + 1)
    for i in range(NP_):
        issue_cast(0, i)
    cast_next[0] = NP_
    ctmp = Fv(0)
    dma(ident[:], cst_d["ident"], [], ["ident"], "c0")
    dma(ones_f[:], cst_d["ones"], [], ["ones_f"], "c1")
    dma(umask_f[:], cst_d["umask"], [], ["umask_f"], "c2")
    dma(invf[:], cst_d["invf"], [], ["invf"], "c3")
    dma(eret[:], cst_d["eret"], [], ["eret"], "c4")
    dma(iota[:], cst_d["iota"], [], ["iota"], "c5")
    dma(fing[:], fing_d, [], ["fing"], "c6")
    copy(ones_b[:], ones_f[:], ["ones_f"], ["ones_b"])
    for q in range(4):
        copy(mask_b[:, q * 128:(q + 1) * 128], umask_f[:], ["umask_f"], ["mask_b"])
    memset(graug[:], 1.0, ["graug"])
    memset(onec[:], 1.0, ["onec"])
    dma(invcnt[:], cst_d["invcnt"], [], ["invcnt"], "c7")

    for t in range(NT):
        dma(posi[:], pos[0:1, t * T:(t + 1) * T].partition_broadcast(128)[:, 0, :], [], ["posi"], "posld")
        copy(Fv(0), posi[:], ["posi"], [("F", 0)])
        ts(Fv(1), Fv(0), invf[:, 0:1], None, ALU.mult, None, [("F", 0), "invf"], [("F", 1)])
        sincos(Fv(1), Fv(2), Fv(3), (Fv(4), Fv(5)), ki[:], [("F", 1)], ("F", 2), ("F", 3), [("F", 4), ("F", 5)], "ki")
        for k8 in range(8):
            dma(Fv(6 + k8), cst_d["dec"][k8], [], [("F", 6 + k8)], f"dec{k8 % 2}")
            src = Fv(3) if (k8 % 2 == 0) else Fv(2)
            tt(Fv(6 + k8), Fv(6 + k8), src, ALU.mult, [("F", 6 + k8), ("F", 2), ("F", 3)], [("F", 6 + k8)])
            dma(rot_d[t, k8], Fv(6 + k8), [("F", 6 + k8)], [("rot", t)], f"rotst{k8 % 2}")

    def pcol(nm, i=0, n=1, rows=128):
        o, _ = PAR[nm]
        return par[0:rows, o + i: o + i + n]

    def rmsnorm(gname, src_key="hT"):
        bk = bank()
        for dc in range(DC):
            activation(B(40 + (dc % 2)), hT[:, dc, :], AF.Square, [("hT", dc)], [("B", 40 + (dc % 2))])
            sq = B(40 + (dc % 2))
            def fn(e, dc=dc, sq=sq, bk=bk):
                return e.matmul(ps[bk][:], lhsT=ones_b[:], rhs=sq, start=(dc == 0), stop=(dc == DC - 1))
            pe(fn, [("B", 40 + (dc % 2)), "ones_b"], [("PS", bk)])
        activation(Fv(0), ps[bk][:], AF.Ln, [("PS", bk)], [("F", 0)], scale=1.0 / D, bias=epsc[:, 0:1])
        activation(Fv(1), Fv(0), AF.Exp, [("F", 0)], [("F", 1)], scale=-0.5)
        return Fv(1), ("F", 1)

    epsc = sb("epsc", [128, 1], F32)
    memset(epsc[:], EPS, ["epsc"])

    def fused_sq(dc):
        sl = 44 + (dc % 4)
        activation(B(sl), hT[:, dc, :], AF.Square, [("hT", dc)], [("B", sl)])

    def fused_mm(dc):
        sl = 44 + (dc % 4)
        pe(lambda e: e.matmul(ps[7][:], lhsT=ones_b[:], rhs=B(sl), start=(dc == 0), stop=(dc == DC - 1)),
           [("B", sl), "ones_b"], [("PS", 7)])

    def fused_rs():
        activation(Fv(0), ps[7][:], AF.Ln, [("PS", 7)], [("F", 0)], scale=1.0 / D, bias=epsc[:, 0:1])
        activation(Fv(1), Fv(0), AF.Exp, [("F", 0)], [("F", 1)], scale=-0.5)
        return Fv(1), ("F", 1)

    def head_norm_gate(o_bank, gsl, ysl, fb=2):
        f0, f1, f2, f3 = fb, fb + 1, fb + 2, fb + 3
        activation(Fv(f0), ps[o_bank][:], AF.Copy, [("PS", o_bank)], [("F", f0)])
        b1 = bank()
        mm_group(ps[b1][:], [(ones_f[:], Fv(f0))], [("F", f0), "ones_f"], [("PS", b1)])
        stt(Fv(f1), ps[b1][:], -1.0 / 128, Fv(f0), ALU.mult, ALU.add, [("PS", b1), ("F", f0)], [("F", f1)])
        activation(Fv(f2), Fv(f1), AF.Square, [("F", f1)], [("F", f2)])
        b2 = bank()
        mm_group(ps[b2][:], [(ones_f[:], Fv(f2))], [("F", f2), "ones_f"], [("PS", b2)])
        activation(Fv(f3), ps[b2][:], AF.Ln, [("PS", b2)], [("F", f3)], scale=1.0 / 128, bias=epsc[:, 0:1])
        activation(Fv(f2), Fv(f3), AF.Exp, [("F", f3)], [("F", f2)], scale=-0.5)
        tt(Fv(f1), Fv(f1), Fv(f2), ALU.mult, [("F", f1), ("F", f2)], [("F", f1)])
        tt(B(ysl), Fv(f1), B(gsl), ALU.mult, [("F", f1), ("B", gsl)], [("B", ysl)])

    def proj_chunk(name, m=128):
        w, wk = nextw(name)
        bk = bank()
        mm_group(ps[bk][0:m, :], [(lhs(w, kc, m), hn[:, kc, :]) for kc in range(DC)],
                 [wk] + [("hn", kc) for kc in range(DC)], [("PS", bk)])
        return bk

    def proj_v(prefix):
        ws = [nextw(f"{prefix}{q}") for q in range(4)]
        for tc in range(4):
            bk = bank()
            pairs = []
            for kc in range(DC):
                w = ws[kc // 4][0]
                pairs.append((hn[:, kc, tc * 128:(tc + 1) * 128], w[:, (kc % 4) * 512:(kc % 4 + 1) * 512]))
            mm_group(ps[bk][:], pairs, [x[1] for x in ws] + [("hn", kc) for kc in range(DC)], [("PS", bk)])
            activation(vtok[:, tc, :], ps[bk][:], AF.Copy, [("PS", bk)], [("vtok", tc)])

    def linattn(br, qs, ks, Ecol, gs, ys, first_tile):
        steps = []
        if first_tile:
            memset(Rst[:, 2 * br:2 * br + 2, :], 0.0, [("Rst", 2 * br), ("Rst", 2 * br + 1)])

        def state_step(n, c):
            si = 2 * br + c
            ri = c * 4 + n
            bk = bank()
            mm_group(ps[bk][:, 0:128], [(B(ks[c])[:, n * 128:(n + 1) * 128], identb[:])],
                     [("B", ks[c]), "identb"], [("PS", bk)])
            activation(ktok[:, c, :], ps[bk][:, 0:128], AF.Copy, [("PS", bk)], [("ktok", c)])
            copy(Rb[:, ri, :], Rst[:, si, :], [("Rst", si)], [("Rb", ri)], eng="pool")
            bk2 = bank()
            mm_group(ps[bk2][:, 0:256], [(ktok[:, c, :], vtok[:, n, c * 256:(c + 1) * 256])],
                     [("ktok", c), ("vtok", n)], [("PS", bk2)])
            tt(Rst[:, si, :], Rst[:, si, :], ps[bk2][:, 0:256], ALU.add, [("Rst", si), ("PS", bk2)], [("Rst", si)])
            eap, ekey = Ecol(c, n)
            ts(Rst[:, si, :], Rst[:, si, :], eap, None, ALU.mult, None, [("Rst", si), ekey], [("Rst", si)])

        hstate = {}

        def head_step(h):
            c, hl = h // 2, h % 2
            r0, r1 = hl * 64, (hl + 1) * 64
            bk = bank()

            def fn(e):
                ins = None
                for n in range(4):
                    ins = e.matmul(ps[bk][:, n * 128:(n + 1) * 128],
                                   lhsT=B(ks[c])[r0:r1, n * 128:(n + 1) * 128],
                                   rhs=B(qs[c])[r0:r1, n * 128:(n + 1) * 128], start=True, stop=True)
                return ins
            pe(fn, [("B", ks[c]), ("B", qs[c])], [("PS", bk)])
            msl = 44 + (h % 2)
            tt(B(msl), ps[bk][:], mask_b[:], ALU.mult, [("PS", bk), "mask_b"], [("B", msl)])
            bo = bank()

            def fn2(e):
                ins = None
                for n in range(4):
                    ri = c * 4 + n
                    e.matmul(ps[bo][:, n * 128:(n + 1) * 128], lhsT=vtok[:, n, h * 128:(h + 1) * 128],
                             rhs=B(msl)[:, n * 128:(n + 1) * 128], start=True, stop=False)
                    ins = e.matmul(ps[bo][:, n * 128:(n + 1) * 128], lhsT=Rb[r0:r1, ri, hl * 128:(hl + 1) * 128],
                                   rhs=B(qs[c])[r0:r1, n * 128:(n + 1) * 128], start=False, stop=True)
                return ins
            pe(fn2, [("B", msl), ("B", qs[c])] + [("vtok", n) for n in range(4)] +
               [("Rb", c * 4 + n) for n in range(4)], [("PS", bo)])
            hstate[h] = bo

        def head_s2(h):
            head_norm_gate(hstate[h], gs[h], ys[h], fb=2 + 4 * (h % 2))

        for n in range(4):
            for c in range(2):
                steps.append((state_step, (n, c)))
        steps += [(head_step, (0,)), (head_step, (1,)), (head_s2, (0,)), (head_step, (2,)), (head_s2, (1,)),
                  (head_step, (3,)), (head_s2, (2,)), (head_s2, (3,))]
        return steps

    def run_steps(*lists):
        lists = [l_ for l_ in lists if l_]
        pos_ = [0] * len(lists)
        total = sum(len(l_) for l_ in lists)
        for _ in range(total):
            best, bi = None, -1
            for i_, l_ in enumerate(lists):
                if pos_[i_] < len(l_):
                    frac = pos_[i_] / len(l_)
                    if best is None or frac < best:
                        best, bi = frac, i_
            f_, args_ = lists[bi][pos_[bi]]
            f_(*args_)
            pos_[bi] += 1

    identb = sb("identb", [128, 128], BF16)
    copy(identb[:], ident[:], ["ident"], ["identb"])

    YA, YC, YB_, YD = 0, 4, 8, 12
    YSL = {0: 0, 1: 8, 2: 4, 3: 12}
    MRG = 16
    QS, KS, GS = (32, 33), (34, 35), (36, 37, 38, 39)

    for l in range(depth):
        P.epoch = l
        dma(par[:], par_d[l], [], ["par"], "parld")
        def M(i):
            return s5m[:, :, i]
        kS = lambda i: ("s5m", i)
        a_re = pcol("a_re", 0, 16); a_im = pcol("a_im", 0, 16); logdt = pcol("logdt", 0, 16)
        activation(M(0), logdt, AF.Exp, ["par"], [kS(0)])
        tt(M(1), a_re, M(0), ALU.mult, ["par", kS(0)], [kS(1)])
        activation(M(2), M(1), AF.Exp, [kS(1)], [kS(2)])
        tt(M(3), a_im, M(0), ALU.mult, ["par", kS(0)], [kS(3)])
        sincos(M(3), M(4), M(5), (M(6), M(7)), s5mi[:], [kS(3)], kS(4), kS(5), [kS(6), kS(7)], "s5mi")
        tt(M(8), M(2), M(5), ALU.mult, [kS(2), kS(5)], [kS(8)])
        tt(M(9), M(2), M(4), ALU.mult, [kS(2), kS(4)], [kS(9)])
        ts(M(10), M(8), -1.0, None, ALU.add, None, [kS(8)], [kS(10)])
        tt(M(11), a_re, a_re, ALU.mult, ["par"], [kS(11)])
        tt(M(12), a_im, a_im, ALU.mult, ["par"], [kS(12)])
        tt(M(11), M(11), M(12), ALU.add, [kS(11), kS(12)], [kS(11)])
        dve(lambda e: e.reciprocal(out=M(12), in_=M(11)), [kS(11)], [kS(12)])
        tt(M(13), M(10), a_re, ALU.mult, [kS(10), "par"], [kS(13)])
        tt(M(14), M(9), a_im, ALU.mult, [kS(9), "par"], [kS(14)])
        tt(M(13), M(13), M(14), ALU.add, [kS(13), kS(14)], [kS(13)])
        tt(M(13), M(13), M(12), ALU.mult, [kS(13), kS(12)], [kS(13)])
        tt(M(14), M(9), a_re, ALU.mult, [kS(9), "par"], [kS(14)])
        tt(M(15), M(10), a_im, ALU.mult, [kS(10), "par"], [kS(15)])
        tt(M(14), M(14), M(15), ALU.subtract, [kS(14), kS(15)], [kS(14)])
        tt(M(14), M(14), M(12), ALU.mult, [kS(14), kS(12)], [kS(14)])
        ts(M(16), M(7), float(T), None, ALU.mult, None, [kS(7)], [kS(16)])
        sincos(M(16), M(17), M(18), (M(19), M(20)), s5mi[:], [kS(16)], kS(17), kS(18), [kS(19), kS(20)], "s5mi")
        for jj in range(16):
            ts(Fv(1), iota[:], M(7)[:, jj:jj + 1], None, ALU.mult, None, ["iota", kS(7)], [("F", 1)])
            sincos(Fv(1), Fv(2), Fv(3), (Fv(4), Fv(5)), ki[:], [("F", 1)], ("F", 2), ("F", 3), [("F", 4), ("F", 5)], "ki")
            dma(rot5_d[jj, 0], Fv(3), [("F", 3)], [("rot5", jj)], "r5st0")
            ts(Fv(2), Fv(2), -1.0, None, ALU.mult, None, [("F", 2)], [("F", 2)])
            dma(rot5_d[jj, 1], Fv(2), [("F", 2)], [("rot5", jj)], "r5st1")
        for jj in range(16):
            for k2, fi in ((0, 13), (1, 14)):
                ts(dgtmp[:], ident[:], M(fi)[:, jj:jj + 1], None, ALU.mult, None, ["ident", kS(fi)], ["dgtmp"])
                bk = bank()
                mm_group(ps[bk][:, k2 * 128:(k2 + 1) * 128], [(ones_f[:], dgtmp[:])], ["ones_f", "dgtmp"], [("PS", bk)])
                copy(Fv(6 + k2)[:, 0:128], ps[bk][:, k2 * 128:(k2 + 1) * 128], [("PS", bk)], [("F", 6 + k2)])
            bre = Fv(10)[:, 0:128]
            bim = Fv(11)[:, 0:128]
            dma(bre, bt_d[l][0][:, jj * 128:(jj + 1) * 128], [], [("F", 10)], "btld")
            dma(bim, bt_d[l][1][:, jj * 128:(jj + 1) * 128], [], [("F", 11)], "btld2")
            fr, fi_ = Fv(6)[:, 0:128], Fv(7)[:, 0:128]
            t0, t1 = Fv(8)[:, 0:128], Fv(9)[:, 0:128]
            tt(t0, fr, bre, ALU.mult, [("F", 6), ("F", 10), ("F", 11)], [("F", 8)])
            tt(t1, fi_, bim, ALU.mult, [("F", 7), ("F", 10), ("F", 11)], [("F", 9)])
            tt(bbT[:, 0, jj, :], t0, t1, ALU.subtract, [("F", 8), ("F", 9)], ["bbT"])
            tt(t0, fr, bim, ALU.mult, [("F", 6), ("F", 10), ("F", 11)], [("F", 8)])
            tt(t1, fi_, bre, ALU.mult, [("F", 7), ("F", 10), ("F", 11)], [("F", 9)])
            tt(bbT[:, 1, jj, :], t0, t1, ALU.add, [("F", 8), ("F", 9)], ["bbT"])
        for oc in range(4):
            ts(dgd[:, oc, :], ident[:], pcol("s5_d", oc), None, ALU.mult, None, ["ident", "par"], ["dgd"])

        for t in range(NT):
            first = (t == 0)
            tsl = slice(t * T, (t + 1) * T)
            src = xT if l == 0 else hscr
            dma(hT[:], src.rearrange("(c p) s -> p c s", p=128)[:, :, tsl],
                [("hscr", t)] if l > 0 else [], [("hT", dc) for dc in range(DC)], "hld")
            if l == 0:
                rs, rsk = rmsnorm("g_mix")
            else:
                dma(Fv(1), rs_d[t], [("rsd", t)], [("F", 1)], "rsld")
                rs, rsk = Fv(1), ("F", 1)
            for dc in range(DC):
                stt(hn[:, dc, :], hT[:, dc, :], pcol("g_mix", dc), rs, ALU.mult, ALU.mult,
                    [("hT", dc), "par", rsk], [("hn", dc)])
            for which, dst, tb in (("RQ", QS, 0), ("RK", KS, 4)):
                for c in range(2):
                    b1 = proj_chunk(f"{which}{c}")
                    b2 = proj_chunk(f"{which}S{c}")
                    dma(Fv(6), rot_d[t, tb + 2 * c], [("rot", t)], [("F", 6)], "rt0")
                    dma(Fv(7), rot_d[t, tb + 2 * c + 1], [("rot", t)], [("F", 7)], "rt1")
                    tt(Fv(8), ps[b1][:], Fv(6), ALU.mult, [("PS", b1), ("F", 6)], [("F", 8)])
                    tt(Fv(9), ps[b2][:], Fv(7), ALU.mult, [("PS", b2), ("F", 7)], [("F", 9)])
                    tt(B(dst[c]), Fv(8), Fv(9), ALU.add, [("F", 8), ("F", 9)], [("B", dst[c])])
            proj_v("RV")
            for c in range(4):
                bk = proj_chunk(f"RG{c}")
                activation(B(GS[c]), ps[bk][:], AF.Silu, [("PS", bk)], [("B", GS[c])])
            run_steps(linattn(0, QS, KS, lambda c, n: (eret[:, c:c + 1], "eret"), GS, [YSL[0] + h for h in range(4)], first))
            bk = proj_chunk("GR", m=16)
            copy(graug[0:16, :], ps[bk][0:16, :], [("PS", bk)], ["graug"])
            EB, EK = Fv(10), Fv(11)
            for tc in range(4):
                bk = bank()
                mm_group(ps[bk][:, 0:256], [(graug[0:17, tc * 128:(tc + 1) * 128], pcol("wgate", 0, 256, rows=17))],
                         ["graug", "par"], [("PS", bk)])
                activation(nla[:], ps[bk][:, 0:256], AF.Exp, [("PS", bk)], ["nla"], scale=-1.0)
                activation(nla[:], nla[:], AF.Ln, ["nla"], ["nla"], bias=onec[:, 0:1])
                for c in range(2):
                    b2 = bank()
                    mm_group(ps[b2][:, 0:128], [(nla[:, c * 128:(c + 1) * 128], umask_f[:])], ["nla", "umask_f"], [("PS", b2)])
                    activation(Fv(10 + c)[:, tc * 128:(tc + 1) * 128], ps[b2][:, 0:128], AF.Exp, [("PS", b2)],
                               [("F", 10 + c)], scale=-1.0 / 16)
                    activation(Fv(12 + c)[:, tc * 128:(tc + 1) * 128], ps[b2][:, 0:128], AF.Exp, [("PS", b2)],
                               [("F", 12 + c)], scale=1.0 / 16)
            for c in range(2):
                bk = proj_chunk(f"GQ{c}")
                stt(B(QS[c]), ps[bk][:], 0.125, Fv(10 + c), ALU.mult, ALU.mult, [("PS", bk), ("F", 10 + c)], [("B", QS[c])])
            for c in range(2):
                bk = proj_chunk(f"GK{c}")
                tt(B(KS[c]), ps[bk][:], Fv(12 + c), ALU.mult, [("PS", bk), ("F", 12 + c)], [("B", KS[c])])
            proj_v("GV")
            for c in range(4):
                bk = proj_chunk(f"GG{c}")
                activation(B(GS[c]), ps[bk][:], AF.Silu, [("PS", bk)], [("B", GS[c])])
            run_steps(linattn(1, QS, KS, lambda c, n: (Fv(10 + c)[:, n * 128 + 127:n * 128 + 128], ("F", 10 + c)), GS,
                              [YSL[2] + h for h in range(4)], first))
            if first:
                memset(pbuf[:, :, 0:16], 0.0, [("pbuf", g) for g in range(4)])
            pbks = []
            for g in range(4):
                bk = proj_chunk(f"PU{g}")
                activation(pbuf[:, g, 16:16 + T], ps[bk][:], AF.Copy, [("PS", bk)], [("pbuf", g)])
            wpl, kpl = nextw("POOLW")
            for g in range(4):
                win = (2, 4, 8, 16)[g]
                cur = pbuf[:, g, :]
                k = ("pbuf", g)
                lvl, lk, lo, sh, pi = cur, k, 0, 1, 0
                while sh < win:
                    dsta = Ft[:, 12 + 2 * pi:14 + 2 * pi, :].rearrange("p a b -> p (a b)")
                    dk = ("F", 12 + 2 * pi)
                    dk2 = ("F", 13 + 2 * pi)
                    tt(dsta[:, lo + sh:16 + T], lvl[:, lo + sh:16 + T], lvl[:, lo:16 + T - sh], ALU.add, [lk], [dk, dk2])
                    lvl, lk, lo = dsta, dk, lo + sh
                    sh *= 2
                    pi ^= 1
                stt(Fv(4), lvl[:, 16:16 + T], 1.0 / win, cur[:, 16:16 + T], ALU.mult, ALU.subtract, [lk, k], [("F", 4)])
                if first:
                    tt(Fv(5)[:, 0:16], lvl[:, 16:32], invcnt[:, g, :], ALU.mult, [lk, "invcnt"], [("F", 5)])
                    tt(Fv(4)[:, 0:16], Fv(5)[:, 0:16], cur[:, 16:32], ALU.subtract, [("F", 5), k, ("F", 4)], [("F", 4)])
                copy(B(58), Fv(4), [("F", 4)], [("B", 58)])
                bk = bank()
                mm_group(ps[bk][:], [(lhs(wpl, g), B(58))], [kpl, ("B", 58)], [("PS", bk)])
                ts(B(YSL[3] + g), ps[bk][:], pcol("pool_scale", g), None, ALU.mult, None, [("PS", bk), "par"], [("B", YSL[3] + g)])
                copy(pbuf[:, g, 0:16], pbuf[:, g, T:T + 16], [k], [k], eng="pool")
            SU = (46, 47, 48, 49)
            for c in range(4):
                bk = proj_chunk(f"SU{c}")
                activation(B(SU[c]), ps[bk][:], AF.Copy, [("PS", bk)], [("B", SU[c])])
            s5w = []
            for i3, nm3 in enumerate(DIRECT):
                dst3 = Bt[:, 32 + 4 * i3:36 + 4 * i3, :].rearrange("p a b -> p (a b)")
                k3 = ("B", 32 + 4 * i3)
                dma(dst3, wb[l][PIDX[nm3]], [("wb", l, PIDX[nm3])], [("B", 32 + 4 * i3 + x3) for x3 in range(4)], f"s5w{i3}")
                s5w.append((dst3, k3))
            (wcre, kcre), (wcim, kcim), (wglu, kglu) = s5w
            if first:
                memset(s5z[:], 0.0, ["s5z"])

            def s5_chunk(oc, jl, wcre=wcre, wcim=wcim, kcre=kcre, kcim=kcim):
                bo = 7
                jj = oc * 4 + jl
                par_ = jj % 2
                TC, TN = (14, 15) if par_ == 0 else (10, 11)
                ZR, ZI = (0, 1) if par_ == 0 else (6, 7)
                dma(Fv(TC), rot5_d[jj, 0], [("rot5", jj)], [("F", TC)], f"r5l0{par_}")
                dma(Fv(TN), rot5_d[jj, 1], [("rot5", jj)], [("F", TN)], f"r5l1{par_}")
                COS, NSN = Fv(TC), Fv(TN)
                br_ = bank(); bi_ = bank()
                mm_group(ps[br_][:], [(bbT[:, 0, jj, :], B(SU[oc]))], ["bbT", ("B", SU[oc])], [("PS", br_)])
                mm_group(ps[bi_][:], [(bbT[:, 1, jj, :], B(SU[oc]))], ["bbT", ("B", SU[oc])], [("PS", bi_)])
                tt(Fv(ZR), ps[br_][:], COS, ALU.mult, [("PS", br_), ("F", TC)], [("F", ZR)])
                tt(Fv(12), ps[bi_][:], NSN, ALU.mult, [("PS", bi_), ("F", TN)], [("F", 12)])
                tt(Fv(ZI), ps[bi_][:], COS, ALU.mult, [("PS", bi_), ("F", TC)], [("F", ZI)])
                tt(Fv(13), ps[br_][:], NSN, ALU.mult, [("PS", br_), ("F", TN)], [("F", 13)])
                tt(Fv(ZR), Fv(ZR), Fv(12), ALU.subtract, [("F", ZR), ("F", 12)], [("F", ZR)])
                tt(Fv(ZI), Fv(ZI), Fv(13), ALU.add, [("F", ZI), ("F", 13)], [("F", ZI)])
                rcol = s5m[:, jj, 2:3]
                for (wsl, k2) in ((ZR, 0), (ZI, 1)):
                    dve(lambda e, wsl=wsl, k2=k2: e.tensor_tensor_scan(
                        out=Fv(wsl), data0=rcol.to_broadcast([128, T]), data1=Fv(wsl),
                        initial=s5z[:, jj, k2:k2 + 1], op0=ALU.mult, op1=ALU.add),
                        [("F", wsl), ("s5m", 2), "s5z"], [("F", wsl)])
                    activation(s5zl[:, jj, k2:k2 + 1], Fv(wsl)[:, T - 1:T], AF.Copy, [("F", wsl)], ["s5zl"])
                XR, XI = 50 + 2 * (jl % 2), 51 + 2 * (jl % 2)
                tt(Fv(8), Fv(ZR), COS, ALU.mult, [("F", ZR), ("F", TC)], [("F", 8)], eng="pool")
                tt(Fv(9), Fv(ZI), NSN, ALU.mult, [("F", ZI), ("F", TN)], [("F", 9)], eng="pool")
                tt(B(XR), Fv(8), Fv(9), ALU.add, [("F", 8), ("F", 9)], [("B", XR)], eng="pool")
                tt(Fv(8), Fv(ZR), NSN, ALU.mult, [("F", ZR), ("F", TN)], [("F", 8)], eng="pool")
                tt(Fv(9), Fv(ZI), COS, ALU.mult, [("F", ZI), ("F", TC)], [("F", 9)], eng="pool")
                tt(B(XI), Fv(8), Fv(9), ALU.subtract, [("F", 8), ("F", 9)], [("B", XI)], eng="pool")

            def s5_chunkB(oc, jl, wcre=wcre, wcim=wcim, kcre=kcre, kcim=kcim):
                bo = 7
                jj = oc * 4 + jl
                XR, XI = 50 + 2 * (jl % 2), 51 + 2 * (jl % 2)

                def fn(e):
                    e.matmul(ps[bo][:], lhsT=lhs(wcre, jj), rhs=B(XR), start=(jl == 0), stop=False)
                    return e.matmul(ps[bo][:], lhsT=lhs(wcim, jj), rhs=B(XI), start=False, stop=False)
                pe(fn, [kcre, kcim, ("B", XR), ("B", XI)], [("PS", bo)])

            def s5_epi(oc):
                bo = 7
                pe(lambda e: e.matmul(ps[bo][:], lhsT=dgd[:, oc, :], rhs=B(SU[oc]), start=False, stop=True),
                   ["dgd", ("B", SU[oc])], [("PS", bo)])
                activation(Fv(12), ps[bo][:], AF.Copy, [("PS", bo)], [("F", 12)])
                tt(Fv(13), Fv(12), Fv(12), ALU.mult, [("F", 12)], [("F", 13)])
                ts(Fv(13), Fv(13), 0.044715, 1.0, ALU.mult, ALU.add, [("F", 13)], [("F", 13)])
                tt(Fv(13), Fv(13), Fv(12), ALU.mult, [("F", 13), ("F", 12)], [("F", 13)])
                activation(Fv(13), Fv(13), AF.Sigmoid, [("F", 13)], [("F", 13)], scale=2.0 * math.sqrt(2.0 / math.pi))
                tt(B(54 + oc), Fv(12), Fv(13), ALU.mult, [("F", 12), ("F", 13)], [("B", 54 + oc)])

            def s5_fin(wglu=wglu, kglu=kglu):
                zr, zi = s5zl[:, :, 0], s5zl[:, :, 1]
                c5, sn5 = s5m[:, :, 18], s5m[:, :, 17]
                t0, t1, t2, t3 = s5zt[:, :, 0], s5zt[:, :, 1], s5zt[:, :, 2], s5zt[:, :, 3]
                tt(t0, zr, c5, ALU.mult, ["s5zl", ("s5m", 18)], [("s5zt", 0)])
                tt(t1, zi, sn5, ALU.mult, ["s5zl", ("s5m", 17)], [("s5zt", 1)])
                tt(t2, zr, sn5, ALU.mult, ["s5zl", ("s5m", 17)], [("s5zt", 2)])
                tt(t3, zi, c5, ALU.mult, ["s5zl", ("s5m", 18)], [("s5zt", 3)])
                tt(s5z[:, :, 0], t0, t1, ALU.subtract, [("s5zt", 0), ("s5zt", 1)], ["s5z"])
                tt(s5z[:, :, 1], t2, t3, ALU.add, [("s5zt", 2), ("s5zt", 3)], ["s5z"])
                for oc in range(4):
                    bk = bank()
                    mm_group(ps[bk][:], [(lhs(wglu, oc * 4 + kc), B(54 + kc)) for kc in range(4)],
                             [kglu] + [("B", 54 + kc) for kc in range(4)], [("PS", bk)])
                    activation(Fv(12), ps[bk][:], AF.Sigmoid, [("PS", bk)], [("F", 12)])
                    tt(B(YSL[1] + oc), Fv(12), B(54 + oc), ALU.mult, [("F", 12), ("B", 54 + oc)], [("B", YSL[1] + oc)])

            s5steps = []
            for g in range(17):
                if g < 16:
                    s5steps.append((s5_chunk, (g // 4, g % 4)))
                if g >= 1:
                    gp = g - 1
                    s5steps.append((s5_chunkB, (gp // 4, gp % 4)))
                    if gp % 4 == 3:
                        s5steps.append((s5_epi, (gp // 4,)))
            s5steps.append((s5_fin, ()))
            def pm_step(dc):
                for gi_, n in enumerate((0, 2, 3)):
                    bk = proj_chunk(f"GATE{n}_{dc}")
                    activation(B(58 + gi_), ps[bk][:], AF.Sigmoid, [("PS", bk)], [("B", 58 + gi_)])
                wbr, kbr = nextw(f"WBRA{dc}")
                for gi_, n in enumerate((0, 2, 3)):
                    bk = bank()
                    mm_group(ps[bk][:], [(lhs(wbr, gi_ * 4 + kc), B(YSL[n] + kc)) for kc in range(4)],
                             [kbr] + [("B", YSL[n] + kc) for kc in range(4)], [("PS", bk)])
                    tt(Fv(2 + gi_), ps[bk][:], B(58 + gi_), ALU.mult, [("PS", bk), ("B", 58 + gi_)], [("F", 2 + gi_)])
                tt(Fv(2), Fv(2), Fv(3), ALU.add, [("F", 2), ("F", 3)], [("F", 2)])
                tt(B(MRG + dc), Fv(2), Fv(4), ALU.add, [("F", 2), ("F", 4)], [("B", MRG + dc)])

            run_steps(s5steps, [(pm_step, (dc,)) for dc in range(DC)])
            for q in range(8):
                wbr, kbr = nextw(f"WBRB{q}")
                for dl in range(2):
                    dc = 2 * q + dl
                    bk = proj_chunk(f"GATE1_{dc}")
                    activation(B(61), ps[bk][:], AF.Sigmoid, [("PS", bk)], [("B", 61)])
                    bk = bank()
                    mm_group(ps[bk][:], [(lhs(wbr, dl * 4 + kc), B(YSL[1] + kc)) for kc in range(4)],
                             [kbr] + [("B", YSL[1] + kc) for kc in range(4)], [("PS", bk)])
                    tt(Fv(2 + dl), ps[bk][:], B(61), ALU.mult, [("PS", bk), ("B", 61)], [("F", 2 + dl)])
                    tt(B(MRG + dc), B(MRG + dc), Fv(2 + dl), ALU.add, [("B", MRG + dc), ("F", 2 + dl)], [("B", MRG + dc)])
            for dc in range(DC):
                w, wk = nextw(f"WOUT{dc}")
                bk = bank()
                mm_group(ps[bk][:], [(lhs(w, kc), B(MRG + kc)) for kc in range(DC)],
                         [wk] + [("B", MRG + kc) for kc in range(DC)], [("PS", bk)])
                if dc > 0:
                    fused_mm(dc - 1)
                tt(hT[:, dc, :], hT[:, dc, :], ps[bk][:], ALU.add, [("hT", dc), ("PS", bk)], [("hT", dc)])
                fused_sq(dc)
            fused_mm(DC - 1)
            if first:
                memset(halo[:], 0.0, [("halo", ch) for ch in range(86)], eng="pool")
            rs, rsk = fused_rs()
            for dc in range(DC):
                stt(hn[:, dc, :], hT[:, dc, :], pcol("g_ffn", dc), rs, ALU.mult, ALU.mult,
                    [("hT", dc), "par", rsk], [("hn", dc)])
            for fc in range(FC):
                accs = []
                for k2, (nm, ch) in enumerate(((f"UPA{fc}", fc), (f"UPV{fc}", FC + fc))):
                    bk = proj_chunk(nm)
                    ui = k2
                    ub = ubuf[:, ui, :]
                    uk = ("ubuf", ui)
                    copy(ub[:, 0:2], halo[:, ch, :], [("halo", ch)], [uk], eng="pool")
                    activation(ub[:, 2:2 + T], ps[bk][:], AF.Copy, [("PS", bk)], [uk])
                    copy(halo[:, ch, :], ub[:, T:T + 2], [uk], [("halo", ch)], eng="pool")
                    acc = Fv(2 + k2)
                    ak = ("F", 2 + k2)
                    ts(acc, ub[:, 2:2 + T], pcol("cw2", ch), pcol("cb", ch), ALU.mult, ALU.add, [uk, "par"], [ak])
                    stt(acc, ub[:, 1:1 + T], pcol("cw1", ch), acc, ALU.mult, ALU.add, [uk, "par", ak], [ak])
                    stt(acc, ub[:, 0:T], pcol("cw0", ch), acc, ALU.mult, ALU.add, [uk, "par", ak], [ak])
                activation(Fv(4), Fv(2), AF.Silu, [("F", 2)], [("F", 4)])
                tt(B(fc), Fv(4), Fv(3), ALU.mult, [("F", 4), ("F", 3)], [("B", fc)])
            for dc in range(DC):
                ws = [nextw(f"WDN{dc}_{q}") for q in range(3)]
                bk = bank()
                mm_group(ps[bk][:], [(lhs(ws[kc // 16][0], kc % 16), B(kc)) for kc in range(FC)],
                         [w[1] for w in ws] + [("B", kc) for kc in range(FC)], [("PS", bk)])
                if dc > 0:
                    fused_mm(dc - 1)
                tt(hT[:, dc, :], hT[:, dc, :], ps[bk][:], ALU.add, [("hT", dc), ("PS", bk)], [("hT", dc)])
                fused_sq(dc)
            fused_mm(DC - 1)
            rs, rsk = fused_rs()
            if l < depth - 1:
                dma(hscr.rearrange("(c p) s -> p c s", p=128)[:, :, tsl], hT[:],
                    [("hT", dc) for dc in range(DC)], [("hscr", t)], "hst")
                dma(rs_d[t], rs, [rsk], [("rsd", t)], "rsst")
            else:
                for dc in range(DC):
                    stt(hT[:, dc, :], hT[:, dc, :], fing[:, dc:dc + 1], rs, ALU.mult, ALU.mult,
                        [("hT", dc), "fing", rsk], [("hT", dc)])
                dma(outT.rearrange("(c p) s -> p c s", p=128)[:, :, tsl], hT[:],
                    [("hT", dc) for dc in range(DC)], [("out", t)], "ost")
    P.op("sp", lambda e: e.nop(), [("out", t) for t in range(NT)], [])

    P.finalize()
    sems = {}
    for st in P.streams:
        nm = "s_" + "_".join(str(x) for x in st)
        sems[st] = es.enter_context(nc.semaphore(nm))
    with nc.Block() as block:
        @block.tensor
        def _(e):
            P.emit("pe", e, sems)

        @block.scalar
        def _(e):
            P.emit("act", e, sems)

        @block.vector
        def _(e):
            P.emit("dve", e, sems)

        @block.gpsimd
        def _(e):
            P.emit("pool", e, sems)

        @block.sync
        def _(e):
            P.emit("sp", e, sems)
    es.close()
    return nc, cst


def make_in_maps(inp, S, depth, nb):
    cst = build_consts()
    shared = {}
    for l in range(depth):
        shared[f"wf{l}"] = build_layer_pieces(inp, l)
        shared[f"par{l}"] = build_layer_params(inp, l)
        shared[f"bt{l}"] = build_bt(inp, l)
    shared["fing"] = np.ascontiguousarray(inp["final_g"].reshape(16, 128).T)
    for k, v in cst.items():
        shared["c_" + k] = np.ascontiguousarray(v)
    maps = []
    for b in range(nb):
        m = dict(shared)
        m["xT"] = np.ascontiguousarray(inp["x"][b, :S].T)
        m["pos"] = np.ascontiguousarray(inp["positions"][b, :S][None, :].astype(np.int32))
        maps.append(m)
    return maps


def kernel(**inputs):
    inp = {k: np.asarray(v) for k, v in inputs.items()}
    Bn, S, _ = inp["x"].shape
    depth = inp["w_in"].shape[0]
    nc, _ = build_program(S, depth)
    maps = make_in_maps(inp, S, depth, Bn)
    res = run_bass_kernel_spmd(nc, maps, core_ids=list(range(Bn)))
    out = np.stack([np.ascontiguousarray(r["outT"].T) for r in res.results], axis=0)
    return out.astype(np.float32)
```

```python
import math
import contextlib
import numpy as np
import concourse.bass as bass
import concourse.mybir as mybir
from concourse.bass_utils import run_bass_kernel_spmd

F32 = mybir.dt.float32
BF16 = mybir.dt.bfloat16
I32 = mybir.dt.int32
AF = mybir.ActivationFunctionType
ALU = mybir.AluOpType

D = 2048
DC = 16
T = 512
DFF = 5504
FC = 43
NIN = 12304
EPS = 1e-6
TWO_PI = 2.0 * math.pi
C1 = 6.28125
C2 = TWO_PI - C1

SAME_ENGINE_SYNC = True
NSLOT = 7
LOOKAHEAD = 3


def piece_names():
    n = []
    n += ["RQ0", "RQS0", "RQ1", "RQS1", "RK0", "RKS0", "RK1", "RKS1"]
    n += ["RV0", "RV1", "RV2", "RV3"]
    n += ["RG0", "RG1", "RG2", "RG3"]
    n += ["GR", "GQ0", "GQ1", "GK0", "GK1"]
    n += ["GV0", "GV1", "GV2", "GV3"]
    n += ["GG0", "GG1", "GG2", "GG3"]
    n += ["PU0", "PU1", "PU2", "PU3", "POOLW"]
    n += ["SU0", "SU1", "SU2", "SU3", "CRE", "CIM", "WGLU"]
    for dc in range(DC):
        n += [f"GATE0_{dc}", f"GATE2_{dc}", f"GATE3_{dc}", f"WBRA{dc}"]
    for q in range(8):
        n += [f"WBRB{q}", f"GATE1_{2 * q}", f"GATE1_{2 * q + 1}"]
    n += [f"WOUT{dc}" for dc in range(DC)]
    for fc in range(FC):
        n += [f"UPA{fc}", f"UPV{fc}"]
    for dc in range(DC):
        n += [f"WDN{dc}_{q}" for q in range(3)]
    return n


DIRECT = ("CRE", "CIM", "WGLU")

PIECES = piece_names()
NP_ = len(PIECES)
PIDX = {nm: i for i, nm in enumerate(PIECES)}


def _lhsT_piece(W, cols):
    K = W.shape[0]
    kc = K // 128
    sub = W[:, cols]
    out = np.zeros((128, 16, 128), np.float32)
    out[:, :kc, :sub.shape[1]] = sub.reshape(kc, 128, -1).transpose(1, 0, 2)
    return out.reshape(128, 2048)


def build_layer_pieces(inp, l):
    w_in = inp["w_in"][l]
    arr = np.zeros((NP_, 128, 2048), np.float32)
    ar = np.arange(128)

    def put(nm, a):
        arr[PIDX[nm]] = a

    hl, half, j = ar // 64, (ar % 64) // 32, ar % 32
    for c in range(2):
        base = (2 * c + hl) * 64
        same = base + half * 32 + j
        swap = base + (1 - half) * 32 + j
        put(f"RQ{c}", _lhsT_piece(w_in, 0 + same))
        put(f"RQS{c}", _lhsT_piece(w_in, 0 + swap))
        put(f"RK{c}", _lhsT_piece(w_in, 256 + same))
        put(f"RKS{c}", _lhsT_piece(w_in, 256 + swap))
    def vpieces(prefix, off):
        Wv = w_in[:, off:off + 512]
        r = Wv.reshape(16, 128, 512).transpose(1, 0, 2)
        for q in range(4):
            put(f"{prefix}{q}", r[:, 4 * q:4 * q + 4, :].reshape(128, 2048))
    vpieces("RV", 512)
    for c in range(4):
        put(f"RG{c}", _lhsT_piece(w_in, 1024 + c * 128 + ar))
        put(f"SU{c}", _lhsT_piece(w_in, 1536 + c * 128 + ar))
        put(f"GG{c}", _lhsT_piece(w_in, 3072 + c * 128 + ar))
        put(f"PU{c}", _lhsT_piece(w_in, 3600 + c * 128 + ar))
    for c in range(2):
        put(f"GQ{c}", _lhsT_piece(w_in, 2048 + c * 128 + ar))
        put(f"GK{c}", _lhsT_piece(w_in, 2304 + c * 128 + ar))
    vpieces("GV", 2560)
    put("GR", _lhsT_piece(w_in, 3584 + np.arange(16)))
    for nm, Cm in (("CRE", inp["s5_c_re"][l]), ("CIM", inp["s5_c_im"][l])):
        a = np.zeros((128, 16, 128), np.float32)
        for jj in range(16):
            for gi in range(2):
                g = 2 * jj + gi
                gl = g % 8
                a[gi * 64:(gi + 1) * 64, jj, gl * 16:(gl + 1) * 16] = Cm[g].T
        put(nm, a.reshape(128, 2048))
    wg = inp["s5_w_glu"][l]
    a = np.zeros((128, 16, 128), np.float32)
    for oc in range(4):
        for kc in range(4):
            a[:, oc * 4 + kc, :] = wg[kc * 128:(kc + 1) * 128, oc * 128:(oc + 1) * 128]
    put("WGLU", a.reshape(128, 2048))
    a = np.zeros((128, 16, 128), np.float32)
    for g in range(4):
        a[:, g, :] = inp["pool_w"][l][g]
    put("POOLW", a.reshape(128, 2048))
    wb = inp["w_branch"][l]
    for dc in range(DC):
        for k in range(4):
            put(f"GATE{k}_{dc}", _lhsT_piece(w_in, 4112 + k * D + dc * 128 + ar))
        a = np.zeros((128, 16, 128), np.float32)
        for ki_, k in enumerate((0, 2, 3)):
            for kc in range(4):
                a[:, ki_ * 4 + kc, :] = wb[k, kc * 128:(kc + 1) * 128, dc * 128:(dc + 1) * 128]
        put(f"WBRA{dc}", a.reshape(128, 2048))
        if dc % 2 == 0:
            a = np.zeros((128, 16, 128), np.float32)
            for dl in range(2):
                for kc in range(4):
                    a[:, dl * 4 + kc, :] = wb[1, kc * 128:(kc + 1) * 128, (dc + dl) * 128:(dc + dl + 1) * 128]
            put(f"WBRB{dc // 2}", a.reshape(128, 2048))
        put(f"WOUT{dc}", _lhsT_piece(inp["w_out"][l], dc * 128 + ar))
    w_up = inp["w_up"][l]
    for fc in range(FC):
        put(f"UPA{fc}", _lhsT_piece(w_up, fc * 128 + ar))
        put(f"UPV{fc}", _lhsT_piece(w_up, DFF + fc * 128 + ar))
    wd = inp["w_down"][l]
    for dc in range(DC):
        for q in range(3):
            k0 = q * 16 * 128
            k1 = min(DFF, k0 + 2048)
            put(f"WDN{dc}_{q}", _lhsT_piece(wd[k0:k1], dc * 128 + ar))
    return arr


def par_layout():
    o = {}
    off = 0
    def add(nm, n):
        nonlocal off
        o[nm] = (off, n)
        off += n
    add("g_mix", 16); add("g_ffn", 16)
    add("cw0", 86); add("cw1", 86); add("cw2", 86); add("cb", 86)
    add("pool_scale", 4); add("s5_d", 4)
    add("a_re", 16); add("a_im", 16); add("logdt", 16)
    add("wgate", 256)
    return o, off


PAR, NPAR = par_layout()


def build_layer_params(inp, l):
    a = np.zeros((128, NPAR), np.float32)
    def put(nm, v):
        o, n = PAR[nm]
        a[:v.shape[0], o:o + n] = v
    put("g_mix", inp["norm_mix_g"][l].reshape(16, 128).T)
    put("g_ffn", inp["norm_ffn_g"][l].reshape(16, 128).T)
    cw = inp["conv_w"][l]
    put("cw0", cw[0].reshape(86, 128).T); put("cw1", cw[1].reshape(86, 128).T); put("cw2", cw[2].reshape(86, 128).T)
    put("cb", inp["conv_b"][l].reshape(86, 128).T)
    put("pool_scale", inp["pool_scale"][l].reshape(4, 128).T)
    put("s5_d", inp["s5_d"][l].reshape(4, 128).T)
    put("a_re", inp["s5_a_re"][l].reshape(16, 128).T)
    put("a_im", inp["s5_a_im"][l].reshape(16, 128).T)
    put("logdt", np.repeat(inp["s5_log_dt"][l], 64).reshape(16, 128).T)
    wg = np.concatenate([inp["gla_w_gate"][l], inp["gla_b_gate"][l][None, :]], axis=0)
    put("wgate", wg)
    return a


def build_bt(inp, l):
    out = np.zeros((2, 128, 16, 128), np.float32)
    for k, Bm in enumerate((inp["s5_b_re"][l], inp["s5_b_im"][l])):
        for jj in range(16):
            for gi in range(2):
                g = 2 * jj + gi
                gl = g % 8
                out[k, gl * 16:(gl + 1) * 16, jj, gi * 64:(gi + 1) * 64] = Bm[g].T
    return out.reshape(2, 128, 2048)


def build_consts():
    c = {}
    ar = np.arange(128)
    c["ident"] = np.eye(128, dtype=np.float32)
    c["ones"] = np.ones((128, 128), np.float32)
    s, t = np.meshgrid(ar, ar, indexing="ij")
    c["umask"] = (s <= t).astype(np.float32)
    j = ar % 32
    c["invf"] = (np.float32(10000.0) ** (-(2 * j).astype(np.float32) / np.float32(64))).astype(np.float32)[:, None]
    sgn = np.where((ar % 64) < 32, -1.0, 1.0)
    tau = np.arange(T) % 128
    dec = np.zeros((8, 128, T), np.float32)
    eret = np.zeros((128, 2), np.float32)
    for cc in range(2):
        h = 2 * cc + ar // 64
        lg = np.log(1.0 - 2.0 ** (-5.0 - h.astype(np.float64)))
        dq = np.exp(lg[:, None] * (tau[None, :] + 1.0))
        dk = 0.125 * np.exp(-lg[:, None] * (tau[None, :] + 1.0))
        dec[2 * cc + 0] = dq
        dec[2 * cc + 1] = dq * sgn[:, None]
        dec[4 + 2 * cc + 0] = dk
        dec[4 + 2 * cc + 1] = dk * sgn[:, None]
        eret[:, cc] = np.exp(128.0 * lg)
    c["dec"] = dec
    c["eret"] = eret
    ic = np.zeros((128, 4, 16), np.float32)
    for g, w in enumerate((2, 4, 8, 16)):
        ic[:, g, :] = 1.0 / np.minimum(np.arange(16) + 1.0, float(w))
    c["invcnt"] = ic
    c["iota"] = np.broadcast_to(np.arange(T, dtype=np.float32)[None, :], (128, T)).copy()
    return c


class Op:
    __slots__ = ("eng", "fn", "deps", "stream", "ordinal", "needs_inc")


class Prog:
    ENGS = ("pe", "act", "dve", "pool", "sp")

    def __init__(self):
        self.ops = {e: [] for e in self.ENGS}
        self.last_w = {}
        self.readers = {}
        self.last_dma = {}
        self.epoch = 0
        self.streams = []
        self._sset = set()

    def op(self, eng, fn, reads=(), writes=(), dma_sem=None):
        o = Op()
        o.eng = eng
        o.fn = fn
        o.needs_inc = False
        o.ordinal = 0
        deps = set()
        for k in reads:
            w = self.last_w.get(k)
            if w is not None:
                deps.add(w)
        for k in writes:
            w = self.last_w.get(k)
            if w is not None:
                deps.add(w)
            for r in self.readers.get(k, ()):
                deps.add(r)
        if dma_sem is not None:
            o.stream = ("dma", dma_sem)
            prev = self.last_dma.get(dma_sem)
            if prev is not None:
                deps.add(prev)
            self.last_dma[dma_sem] = o
        else:
            o.stream = (eng, self.epoch)
        if o.stream not in self._sset:
            self._sset.add(o.stream)
            self.streams.append(o.stream)
        deps.discard(o)
        o.deps = deps
        for k in writes:
            self.last_w[k] = o
            self.readers[k] = []
        for k in reads:
            self.readers.setdefault(k, []).append(o)
        self.ops[eng].append(o)
        return o

    @staticmethod
    def need_wait(o, d):
        if d.stream[0] == "dma":
            return True
        if d.eng == o.eng:
            if o.eng == "pe":
                return False
            if o.eng == "pool":
                return True
            return SAME_ENGINE_SYNC
        return True

    def finalize(self):
        for e in self.ENGS:
            for o in self.ops[e]:
                for d in o.deps:
                    if self.need_wait(o, d):
                        d.needs_inc = True
        cnt = {}
        for e in self.ENGS:
            for o in self.ops[e]:
                if o.stream[0] == "dma":
                    cnt[o.stream] = cnt.get(o.stream, 0) + 16
                    o.ordinal = cnt[o.stream]
                    o.needs_inc = True
                elif o.needs_inc:
                    cnt[o.stream] = cnt.get(o.stream, 0) + 1
                    o.ordinal = cnt[o.stream]
        self.maxcount = cnt

    def emit(self, eng_name, e, sems):
        seen = {}
        nwait = 0
        for o in self.ops[eng_name]:
            need = {}
            for d in o.deps:
                if self.need_wait(o, d):
                    if d.ordinal > need.get(d.stream, 0):
                        need[d.stream] = d.ordinal
            for st, val in need.items():
                if seen.get(st, 0) >= val:
                    continue
                e.wait_ge(sems[st], val)
                seen[st] = val
                nwait += 1
            ins = o.fn(e)
            if o.needs_inc:
                ins.then_inc(sems[o.stream], 16 if o.stream[0] == "dma" else 1)
        return nwait


def build_program(S, depth, flags=None):
    flags = flags or {}
    NT = S // T
    nc = bass.Bass("TRN2", target_bir_lowering=False, dynamic_dma_scratch_size=1024)
    P = Prog()
    es = contextlib.ExitStack()

    def dram_in(name, shape, dt=F32):
        return nc.dram_tensor(name, list(shape), dt, kind="ExternalInput").ap()

    xT = dram_in("xT", [D, S])
    pos = dram_in("pos", [1, S], I32)
    wf = [dram_in(f"wf{l}", [NP_, 128, 2048]) for l in range(depth)]
    par_d = [dram_in(f"par{l}", [128, NPAR]) for l in range(depth)]
    bt_d = [dram_in(f"bt{l}", [2, 128, 2048]) for l in range(depth)]
    fing_d = dram_in("fing", [128, 16])
    cst = build_consts()
    cst_d = {k: dram_in("c_" + k, v.shape) for k, v in cst.items()}
    outT = nc.dram_tensor("outT", [D, S], F32, kind="ExternalOutput").ap()
    wb = [nc.dram_tensor(f"wb{l}", [NP_, 128, 2048], BF16, kind="Internal").ap() for l in range(depth)]
    hscr = nc.dram_tensor("hscr", [D, S], F32, kind="Internal").ap()
    rot_d = nc.dram_tensor("rot", [NT, 8, 128, T], F32, kind="Internal").ap()
    rot5_d = nc.dram_tensor("rot5", [16, 2, 128, T], F32, kind="Internal").ap()
    rs_d = nc.dram_tensor("rsd", [NT, 128, T], F32, kind="Internal").ap()

    def sb(name, shape, dt):
        return es.enter_context(nc.sbuf_tensor(name, list(shape), dt))

    ring = sb("ring", [128, NSLOT, 2048], BF16)
    NB = 62
    NF = 16
    Bt = sb("Bt", [128, NB, T], BF16)
    Ft = sb("Ft", [128, NF, T], F32)
    hT = sb("hT", [128, DC, T], F32)
    hn = sb("hn", [128, DC, T], BF16)
    par = sb("par", [128, NPAR], F32)
    fing = sb("fing_s", [128, 16], F32)
    ident = sb("ident", [128, 128], F32)
    ones_f = sb("ones_f", [128, 128], F32)
    ones_b = sb("ones_b", [128, 128], BF16)
    umask_f = sb("umask_f", [128, 128], F32)
    mask_b = sb("mask_b", [128, T], BF16)
    invf = sb("invf", [128, 1], F32)
    eret = sb("eret", [128, 2], F32)
    iota = sb("iota", [128, T], F32)
    posi = sb("posi", [128, T], I32)
    ki = sb("ki", [128, T], I32)
    halo = sb("halo", [128, 86, 2], F32)
    ubuf = sb("ubuf", [128, 2, T + 2], F32)
    pbuf = sb("pbuf", [128, 4, 16 + T], F32)
    Rst = sb("Rst", [128, 4, 256], F32)
    Rb = sb("Rb", [128, 8, 256], BF16)
    ktok = sb("ktok", [128, 2, 128], BF16)
    vtok = sb("vtok", [128, 4, T], BF16)
    graug = sb("graug", [32, T], F32)
    nla = sb("nla", [128, 256], F32)
    s5m = sb("s5m", [128, 16, 26], F32)
    s5z = sb("s5z", [128, 16, 2], F32)
    s5zt = sb("s5zt", [128, 16, 4], F32)
    s5zl = sb("s5zl", [128, 16, 2], F32)
    bbT = sb("bbT", [128, 2, 16, 128], BF16)
    dgd = sb("dgd", [128, 4, 128], BF16)
    dgtmp = sb("dgtmp", [128, 128], F32)
    s5mi = sb("s5mi", [128, 16], I32)
    onec = sb("onec", [128, 1], F32)
    invcnt = sb("invcnt", [128, 4, 16], F32)
    ps = [es.enter_context(nc.psum_tensor(f"ps{i}", [128, T], F32)) for i in range(8)]

    bank_ctr = [0]

    def bank():
        b = bank_ctr[0] % 7
        bank_ctr[0] += 1
        return b

    def B(i):
        return Bt[:, i, :]

    def Fv(i):
        return Ft[:, i, :]

    def dma(out_ap, in_ap, reads, writes, sem, eng="sp", **kw):
        return P.op(eng, lambda e: e.dma_start(out=out_ap, in_=in_ap, **kw), reads, writes, dma_sem=sem)

    def dve(fn, reads, writes):
        return P.op("dve", fn, reads, writes)

    def act(fn, reads, writes):
        return P.op("act", fn, reads, writes)

    def pe(fn, reads, writes):
        return P.op("pe", fn, reads, writes)

    def mm_group(out_ap, pairs, reads, writes):
        def fn(e):
            n = len(pairs)
            ins = None
            for i, (l, r) in enumerate(pairs):
                ins = e.matmul(out_ap, lhsT=l, rhs=r, start=(i == 0), stop=(i == n - 1))
            return ins
        return pe(fn, reads, writes)

    def activation(out_ap, in_ap, func, reads, writes, scale=1.0, bias=None):
        def fn(e):
            if bias is None:
                return e.activation(out=out_ap, in_=in_ap, func=func, scale=scale)
            return e.activation(out=out_ap, in_=in_ap, func=func, scale=scale, bias=bias)
        return act(fn, reads, writes)

    def tt(out_ap, a, b, op, reads, writes, eng="dve"):
        return P.op(eng, lambda e: e.tensor_tensor(out=out_ap, in0=a, in1=b, op=op), reads, writes)

    def ts(out_ap, a, s1, s2, op0, op1, reads, writes, eng="dve"):
        if s2 is None:
            return P.op(eng, lambda e: e.tensor_scalar(out=out_ap, in0=a, scalar1=s1, scalar2=None, op0=op0), reads, writes)
        return P.op(eng, lambda e: e.tensor_scalar(out=out_ap, in0=a, scalar1=s1, scalar2=s2, op0=op0, op1=op1), reads, writes)

    def stt(out_ap, a, s, b, op0, op1, reads, writes):
        return dve(lambda e: e.scalar_tensor_tensor(out=out_ap, in0=a, scalar=s, in1=b, op0=op0, op1=op1), reads, writes)

    def copy(out_ap, in_ap, reads, writes, eng="dve"):
        return P.op(eng, lambda e: e.tensor_copy(out=out_ap, in_=in_ap), reads, writes)

    def memset(ap, val, writes, eng="dve"):
        return P.op(eng, lambda e: e.memset(ap, val), (), writes)

    seq = []
    for l in range(depth):
        for t in range(NT):
            for i in range(NP_):
                if PIECES[i] not in DIRECT:
                    seq.append((l, i))
    NRING = NP_ - len(DIRECT)
    wstate = {"next_load": 0, "next_use": 0}

    def issue_load():
        k = wstate["next_load"]
        if k >= len(seq):
            return
        l, i = seq[k]
        slot = k % NSLOT
        dma(ring[:, slot, :], wb[l][i], [("wb", l, i)], [("ring", slot)], f"ring{slot}")
        wstate["next_load"] = k + 1

    def nextw(name):
        k = wstate["next_use"]
        l, i = seq[k]
        assert PIECES[i] == name, (PIECES[i], name)
        while wstate["next_load"] <= min(k + LOOKAHEAD, len(seq) - 1):
            issue_load()
        wstate["next_use"] = k + 1
        slot = k % NSLOT
        if l + 1 < depth:
            tt_ = k // NRING - l * NT
            if NT > 1:
                target = min(NP_, ((tt_ * NP_ + i + 1) * NP_) // ((NT - 1) * NP_) + 1)
            else:
                target = NP_
            while cast_next[l + 1] < target:
                issue_cast(l + 1, cast_next[l + 1])
                cast_next[l + 1] += 1
        return ring[:, slot, :], ("ring", slot)

    def lhs(wap, kc, m=128):
        return wap[:, kc * 128: kc * 128 + m]

    def sincos(ang, sin_out, cos_out, tmp, tmpi, keys_in, k_sin, k_cos, k_tmp, k_tmpi, shape_slice=None):
        t0, t1 = tmp
        ts(t0, ang, 1.0 / TWO_PI, None, ALU.mult, None, keys_in, [k_tmp[0]])
        copy(tmpi, t0, [k_tmp[0]], [k_tmpi])
        copy(t0, tmpi, [k_tmpi], [k_tmp[0]])
        stt(t1, t0, -C1, ang, ALU.mult, ALU.add, [k_tmp[0]] + list(keys_in), [k_tmp[1]])
        stt(t1, t0, -C2, t1, ALU.mult, ALU.add, [k_tmp[0], k_tmp[1]], [k_tmp[1]])
        ts(t0, t1, math.pi, TWO_PI, ALU.is_gt, ALU.mult, [k_tmp[1]], [k_tmp[0]])
        tt(t1, t1, t0, ALU.subtract, [k_tmp[0], k_tmp[1]], [k_tmp[1]])
        ts(t0, t1, -math.pi, TWO_PI, ALU.is_lt, ALU.mult, [k_tmp[1]], [k_tmp[0]])
        tt(t1, t1, t0, ALU.add, [k_tmp[0], k_tmp[1]], [k_tmp[1]])
        ts(t0, t1, math.pi / 2, None, ALU.add, None, [k_tmp[1]], [k_tmp[0]])
        ts(cos_out, t0, math.pi, TWO_PI, ALU.is_gt, ALU.mult, [k_tmp[0]], [k_cos])
        tt(t0, t0, cos_out, ALU.subtract, [k_tmp[0], k_cos], [k_tmp[0]])
        LIM = 3.1415925
        ts(t0, t0, -LIM, LIM, ALU.max, ALU.min, [k_tmp[0]], [k_tmp[0]])
        ts(t1, t1, -LIM, LIM, ALU.max, ALU.min, [k_tmp[1]], [k_tmp[1]])
        activation(sin_out, t1, AF.Sin, [k_tmp[1]], [k_sin])
        activation(cos_out, t0, AF.Sin, [k_tmp[0]], [k_cos])

    cast_state = {"k": 0}

    def issue_cast(l, i):
        k = cast_state["k"]
        dma(wb[l][i], wf[l][i], [], [("wb", l, i)], f"cast{k % 4}", eng="pool", max_dma_last_dim=8192)
        cast_state["k"] = k + 1

    cast_next = [0] * (depth + 1)
    for i in range(NP_):
        issue_cast(0, i)
    cast_next[0] = NP_
    ctmp = Fv(0)
    dma(ident[:], cst_d["ident"], [], ["ident"], "c0")
    dma(ones_f[:], cst_d["ones"], [], ["ones_f"], "c1")
    dma(umask_f[:], cst_d["umask"], [], ["umask_f"], "c2")
    dma(invf[:], cst_d["invf"], [], ["invf"], "c3")
    dma(eret[:], cst_d["eret"], [], ["eret"], "c4")
    dma(iota[:], cst_d["iota"], [], ["iota"], "c5")
    dma(fing[:], fing_d, [], ["fing"], "c6")
    copy(ones_b[:], ones_f[:], ["ones_f"], ["ones_b"])
    for q in range(4):
        copy(mask_b[:, q * 128:(q + 1) * 128], umask_f[:], ["umask_f"], ["mask_b"])
    memset(graug[:], 1.0, ["graug"])
    memset(onec[:], 1.0, ["onec"])
    dma(invcnt[:], cst_d["invcnt"], [], ["invcnt"], "c7")

    for t in range(NT):
        dma(posi[:], pos[0:1, t * T:(t + 1) * T].partition_broadcast(128)[:, 0, :], [], ["posi"], "posld")
        copy(Fv(0), posi[:], ["posi"], [("F", 0)])
        ts(Fv(1), Fv(0), invf[:, 0:1], None, ALU.mult, None, [("F", 0), "invf"], [("F", 1)])
        sincos(Fv(1), Fv(2), Fv(3), (Fv(4), Fv(5)), ki[:], [("F", 1)], ("F", 2), ("F", 3), [("F", 4), ("F", 5)], "ki")
        for k8 in range(8):
            dma(Fv(6 + k8), cst_d["dec"][k8], [], [("F", 6 + k8)], f"dec{k8 % 2}")
            src = Fv(3) if (k8 % 2 == 0) else Fv(2)
            tt(Fv(6 + k8), Fv(6 + k8), src, ALU.mult, [("F", 6 + k8), ("F", 2), ("F", 3)], [("F", 6 + k8)])
            dma(rot_d[t, k8], Fv(6 + k8), [("F", 6 + k8)], [("rot", t)], f"rotst{k8 % 2}")

    def pcol(nm, i=0, n=1, rows=128):
        o, _ = PAR[nm]
        return par[0:rows, o + i: o + i + n]

    def rmsnorm(gname, src_key="hT"):
        bk = bank()
        for dc in range(DC):
            activation(B(40 + (dc % 2)), hT[:, dc, :], AF.Square, [("hT", dc)], [("B", 40 + (dc % 2))])
            sq = B(40 + (dc % 2))
            def fn(e, dc=dc, sq=sq, bk=bk):
                return e.matmul(ps[bk][:], lhsT=ones_b[:], rhs=sq, start=(dc == 0), stop=(dc == DC - 1))
            pe(fn, [("B", 40 + (dc % 2)), "ones_b"], [("PS", bk)])
        activation(Fv(0), ps[bk][:], AF.Ln, [("PS", bk)], [("F", 0)], scale=1.0 / D, bias=epsc[:, 0:1])
        activation(Fv(1), Fv(0), AF.Exp, [("F", 0)], [("F", 1)], scale=-0.5)
        return Fv(1), ("F", 1)

    epsc = sb("epsc", [128, 1], F32)
    memset(epsc[:], EPS, ["epsc"])

    def fused_sq(dc):
        sl = 44 + (dc % 4)
        activation(B(sl), hT[:, dc, :], AF.Square, [("hT", dc)], [("B", sl)])

    def fused_mm(dc):
        sl = 44 + (dc % 4)
        pe(lambda e: e.matmul(ps[7][:], lhsT=ones_b[:], rhs=B(sl), start=(dc == 0), stop=(dc == DC - 1)),
           [("B", sl), "ones_b"], [("PS", 7)])

    def fused_rs():
        activation(Fv(0), ps[7][:], AF.Ln, [("PS", 7)], [("F", 0)], scale=1.0 / D, bias=epsc[:, 0:1])
        activation(Fv(1), Fv(0), AF.Exp, [("F", 0)], [("F", 1)], scale=-0.5)
        return Fv(1), ("F", 1)

    def head_norm_gate(o_bank, gsl, ysl, fb=2):
        f0, f1, f2, f3 = fb, fb + 1, fb + 2, fb + 3
        activation(Fv(f0), ps[o_bank][:], AF.Copy, [("PS", o_bank)], [("F", f0)])
        b1 = bank()
        mm_group(ps[b1][:], [(ones_f[:], Fv(f0))], [("F", f0), "ones_f"], [("PS", b1)])
        stt(Fv(f1), ps[b1][:], -1.0 / 128, Fv(f0), ALU.mult, ALU.add, [("PS", b1), ("F", f0)], [("F", f1)])
        activation(Fv(f2), Fv(f1), AF.Square, [("F", f1)], [("F", f2)])
        b2 = bank()
        mm_group(ps[b2][:], [(ones_f[:], Fv(f2))], [("F", f2), "ones_f"], [("PS", b2)])
        activation(Fv(f3), ps[b2][:], AF.Ln, [("PS", b2)], [("F", f3)], scale=1.0 / 128, bias=epsc[:, 0:1])
        activation(Fv(f2), Fv(f3), AF.Exp, [("F", f3)], [("F", f2)], scale=-0.5)
        tt(Fv(f1), Fv(f1), Fv(f2), ALU.mult, [("F", f1), ("F", f2)], [("F", f1)])
        tt(B(ysl), Fv(f1), B(gsl), ALU.mult, [("F", f1), ("B", gsl)], [("B", ysl)])

    def proj_chunk(name, m=128, split=False):
        w, wk = nextw(name)
        bk = bank()
        if split:
            for kc in range(DC):
                pe(lambda e, kc=kc: e.matmul(ps[bk][0:m, :], lhsT=lhs(w, kc, m), rhs=hn[:, kc, :],
                                             start=(kc == 0), stop=(kc == DC - 1)),
                   [wk, ("hn", kc)], [("PS", bk)])
        else:
            mm_group(ps[bk][0:m, :], [(lhs(w, kc, m), hn[:, kc, :]) for kc in range(DC)],
                     [wk] + [("hn", kc) for kc in range(DC)], [("PS", bk)])
        return bk

    def proj_v(prefix):
        ws = [nextw(f"{prefix}{q}") for q in range(4)]
        for tc in range(4):
            bk = bank()
            pairs = []
            for kc in range(DC):
                w = ws[kc // 4][0]
                pairs.append((hn[:, kc, tc * 128:(tc + 1) * 128], w[:, (kc % 4) * 512:(kc % 4 + 1) * 512]))
            mm_group(ps[bk][:], pairs, [x[1] for x in ws] + [("hn", kc) for kc in range(DC)], [("PS", bk)])
            activation(vtok[:, tc, :], ps[bk][:], AF.Copy, [("PS", bk)], [("vtok", tc)])

    def linattn(br, qs, ks, Ecol, gs, ys, first_tile):
        steps = []
        if first_tile:
            memset(Rst[:, 2 * br:2 * br + 2, :], 0.0, [("Rst", 2 * br), ("Rst", 2 * br + 1)])

        def state_step(n, c):
            si = 2 * br + c
            ri = c * 4 + n
            bk = bank()
            mm_group(ps[bk][:, 0:128], [(B(ks[c])[:, n * 128:(n + 1) * 128], identb[:])],
                     [("B", ks[c]), "identb"], [("PS", bk)])
            activation(ktok[:, c, :], ps[bk][:, 0:128], AF.Copy, [("PS", bk)], [("ktok", c)])
            copy(Rb[:, ri, :], Rst[:, si, :], [("Rst", si)], [("Rb", ri)], eng="pool")
            bk2 = bank()
            mm_group(ps[bk2][:, 0:256], [(ktok[:, c, :], vtok[:, n, c * 256:(c + 1) * 256])],
                     [("ktok", c), ("vtok", n)], [("PS", bk2)])
            tt(Rst[:, si, :], Rst[:, si, :], ps[bk2][:, 0:256], ALU.add, [("Rst", si), ("PS", bk2)], [("Rst", si)])
            eap, ekey = Ecol(c, n)
            ts(Rst[:, si, :], Rst[:, si, :], eap, None, ALU.mult, None, [("Rst", si), ekey], [("Rst", si)])

        hstate = {}

        def head_step(h):
            c, hl = h // 2, h % 2
            r0, r1 = hl * 64, (hl + 1) * 64
            bk = bank()

            def fn(e):
                ins = None
                for n in range(4):
                    ins = e.matmul(ps[bk][:, n * 128:(n + 1) * 128],
                                   lhsT=B(ks[c])[r0:r1, n * 128:(n + 1) * 128],
                                   rhs=B(qs[c])[r0:r1, n * 128:(n + 1) * 128], start=True, stop=True)
                return ins
            pe(fn, [("B", ks[c]), ("B", qs[c])], [("PS", bk)])
            msl = 44 + (h % 2)
            tt(B(msl), ps[bk][:], mask_b[:], ALU.mult, [("PS", bk), "mask_b"], [("B", msl)])
            bo = bank()

            def fn2(e):
                ins = None
                for n in range(4):
                    ri = c * 4 + n
                    e.matmul(ps[bo][:, n * 128:(n + 1) * 128], lhsT=vtok[:, n, h * 128:(h + 1) * 128],
                             rhs=B(msl)[:, n * 128:(n + 1) * 128], start=True, stop=False)
                    ins = e.matmul(ps[bo][:, n * 128:(n + 1) * 128], lhsT=Rb[r0:r1, ri, hl * 128:(hl + 1) * 128],
                                   rhs=B(qs[c])[r0:r1, n * 128:(n + 1) * 128], start=False, stop=True)
                return ins
            pe(fn2, [("B", msl), ("B", qs[c])] + [("vtok", n) for n in range(4)] +
               [("Rb", c * 4 + n) for n in range(4)], [("PS", bo)])
            hstate[h] = bo

        def head_s2(h):
            head_norm_gate(hstate[h], gs[h], ys[h], fb=2 + 4 * (h % 2))

        for n in range(4):
            for c in range(2):
                steps.append((state_step, (n, c)))
        steps += [(head_step, (0,)), (head_step, (1,)), (head_s2, (0,)), (head_step, (2,)), (head_s2, (1,)),
                  (head_step, (3,)), (head_s2, (2,)), (head_s2, (3,))]
        return steps

    def run_steps(*lists):
        lists = [l_ for l_ in lists if l_]
        pos_ = [0] * len(lists)
        total = sum(len(l_) for l_ in lists)
        for _ in range(total):
            best, bi = None, -1
            for i_, l_ in enumerate(lists):
                if pos_[i_] < len(l_):
                    frac = pos_[i_] / len(l_)
                    if best is None or frac < best:
                        best, bi = frac, i_
            f_, args_ = lists[bi][pos_[bi]]
            f_(*args_)
            pos_[bi] += 1

    identb = sb("identb", [128, 128], BF16)
    copy(identb[:], ident[:], ["ident"], ["identb"])

    YA, YC, YB_, YD = 0, 4, 8, 12
    YSL = {0: 0, 1: 8, 2: 4, 3: 12}
    MRG = 16
    QS, KS, GS = (32, 33), (34, 35), (36, 37, 38, 39)

    for l in range(depth):
        P.epoch = l
        dma(par[:], par_d[l], [], ["par"], "parld")
        def M(i):
            return s5m[:, :, i]
        kS = lambda i: ("s5m", i)
        a_re = pcol("a_re", 0, 16); a_im = pcol("a_im", 0, 16); logdt = pcol("logdt", 0, 16)
        activation(M(0), logdt, AF.Exp, ["par"], [kS(0)])
        tt(M(1), a_re, M(0), ALU.mult, ["par", kS(0)], [kS(1)])
        activation(M(2), M(1), AF.Exp, [kS(1)], [kS(2)])
        tt(M(3), a_im, M(0), ALU.mult, ["par", kS(0)], [kS(3)])
        sincos(M(3), M(4), M(5), (M(6), M(7)), s5mi[:], [kS(3)], kS(4), kS(5), [kS(6), kS(7)], "s5mi")
        tt(M(8), M(2), M(5), ALU.mult, [kS(2), kS(5)], [kS(8)])
        tt(M(9), M(2), M(4), ALU.mult, [kS(2), kS(4)], [kS(9)])
        ts(M(10), M(8), -1.0, None, ALU.add, None, [kS(8)], [kS(10)])
        tt(M(11), a_re, a_re, ALU.mult, ["par"], [kS(11)])
        tt(M(12), a_im, a_im, ALU.mult, ["par"], [kS(12)])
        tt(M(11), M(11), M(12), ALU.add, [kS(11), kS(12)], [kS(11)])
        dve(lambda e: e.reciprocal(out=M(12), in_=M(11)), [kS(11)], [kS(12)])
        tt(M(13), M(10), a_re, ALU.mult, [kS(10), "par"], [kS(13)])
        tt(M(14), M(9), a_im, ALU.mult, [kS(9), "par"], [kS(14)])
        tt(M(13), M(13), M(14), ALU.add, [kS(13), kS(14)], [kS(13)])
        tt(M(13), M(13), M(12), ALU.mult, [kS(13), kS(12)], [kS(13)])
        tt(M(14), M(9), a_re, ALU.mult, [kS(9), "par"], [kS(14)])
        tt(M(15), M(10), a_im, ALU.mult, [kS(10), "par"], [kS(15)])
        tt(M(14), M(14), M(15), ALU.subtract, [kS(14), kS(15)], [kS(14)])
        tt(M(14), M(14), M(12), ALU.mult, [kS(14), kS(12)], [kS(14)])
        ts(M(16), M(7), float(T), None, ALU.mult, None, [kS(7)], [kS(16)])
        sincos(M(16), M(17), M(18), (M(19), M(20)), s5mi[:], [kS(16)], kS(17), kS(18), [kS(19), kS(20)], "s5mi")
        for jj in range(16):
            ts(Fv(1), iota[:], M(7)[:, jj:jj + 1], None, ALU.mult, None, ["iota", kS(7)], [("F", 1)])
            sincos(Fv(1), Fv(2), Fv(3), (Fv(4), Fv(5)), ki[:], [("F", 1)], ("F", 2), ("F", 3), [("F", 4), ("F", 5)], "ki")
            dma(rot5_d[jj, 0], Fv(3), [("F", 3)], [("rot5", jj)], "r5st0")
            ts(Fv(2), Fv(2), -1.0, None, ALU.mult, None, [("F", 2)], [("F", 2)])
            dma(rot5_d[jj, 1], Fv(2), [("F", 2)], [("rot5", jj)], "r5st1")
        for jj in range(16):
            for k2, fi in ((0, 13), (1, 14)):
                ts(dgtmp[:], ident[:], M(fi)[:, jj:jj + 1], None, ALU.mult, None, ["ident", kS(fi)], ["dgtmp"])
                bk = bank()
                mm_group(ps[bk][:, k2 * 128:(k2 + 1) * 128], [(ones_f[:], dgtmp[:])], ["ones_f", "dgtmp"], [("PS", bk)])
                copy(Fv(6 + k2)[:, 0:128], ps[bk][:, k2 * 128:(k2 + 1) * 128], [("PS", bk)], [("F", 6 + k2)])
            bre = Fv(10)[:, 0:128]
            bim = Fv(11)[:, 0:128]
            dma(bre, bt_d[l][0][:, jj * 128:(jj + 1) * 128], [], [("F", 10)], "btld")
            dma(bim, bt_d[l][1][:, jj * 128:(jj + 1) * 128], [], [("F", 11)], "btld2")
            fr, fi_ = Fv(6)[:, 0:128], Fv(7)[:, 0:128]
            t0, t1 = Fv(8)[:, 0:128], Fv(9)[:, 0:128]
            tt(t0, fr, bre, ALU.mult, [("F", 6), ("F", 10), ("F", 11)], [("F", 8)])
            tt(t1, fi_, bim, ALU.mult, [("F", 7), ("F", 10), ("F", 11)], [("F", 9)])
            tt(bbT[:, 0, jj, :], t0, t1, ALU.subtract, [("F", 8), ("F", 9)], ["bbT"])
            tt(t0, fr, bim, ALU.mult, [("F", 6), ("F", 10), ("F", 11)], [("F", 8)])
            tt(t1, fi_, bre, ALU.mult, [("F", 7), ("F", 10), ("F", 11)], [("F", 9)])
            tt(bbT[:, 1, jj, :], t0, t1, ALU.add, [("F", 8), ("F", 9)], ["bbT"])
        for oc in range(4):
            ts(dgd[:, oc, :], ident[:], pcol("s5_d", oc), None, ALU.mult, None, ["ident", "par"], ["dgd"])

        for t in range(NT):
            first = (t == 0)
            tsl = slice(t * T, (t + 1) * T)
            src = xT if l == 0 else hscr
            dma(hT[:], src.rearrange("(c p) s -> p c s", p=128)[:, :, tsl],
                [("hscr", t)] if l > 0 else [], [("hT", dc) for dc in range(DC)], "hld")
            if l == 0:
                rs, rsk = rmsnorm("g_mix")
            else:
                dma(Fv(1), rs_d[t], [("rsd", t)], [("F", 1)], "rsld")
                rs, rsk = Fv(1), ("F", 1)
            for dc in range(DC):
                stt(hn[:, dc, :], hT[:, dc, :], pcol("g_mix", dc), rs, ALU.mult, ALU.mult,
                    [("hT", dc), "par", rsk], [("hn", dc)])
            for which, dst, tb in (("RQ", QS, 0), ("RK", KS, 4)):
                for c in range(2):
                    b1 = proj_chunk(f"{which}{c}", split=(which == "RQ" and c == 0))
                    b2 = proj_chunk(f"{which}S{c}")
                    dma(Fv(6), rot_d[t, tb + 2 * c], [("rot", t)], [("F", 6)], "rt0")
                    dma(Fv(7), rot_d[t, tb + 2 * c + 1], [("rot", t)], [("F", 7)], "rt1")
                    tt(Fv(8), ps[b1][:], Fv(6), ALU.mult, [("PS", b1), ("F", 6)], [("F", 8)])
                    tt(Fv(9), ps[b2][:], Fv(7), ALU.mult, [("PS", b2), ("F", 7)], [("F", 9)])
                    tt(B(dst[c]), Fv(8), Fv(9), ALU.add, [("F", 8), ("F", 9)], [("B", dst[c])])
            proj_v("RV")
            for c in range(4):
                bk = proj_chunk(f"RG{c}")
                activation(B(GS[c]), ps[bk][:], AF.Silu, [("PS", bk)], [("B", GS[c])])
            run_steps(linattn(0, QS, KS, lambda c, n: (eret[:, c:c + 1], "eret"), GS, [YSL[0] + h for h in range(4)], first))
            bk = proj_chunk("GR", m=16)
            copy(graug[0:16, :], ps[bk][0:16, :], [("PS", bk)], ["graug"])
            EB, EK = Fv(10), Fv(11)
            for tc in range(4):
                bk = bank()
                mm_group(ps[bk][:, 0:256], [(graug[0:17, tc * 128:(tc + 1) * 128], pcol("wgate", 0, 256, rows=17))],
                         ["graug", "par"], [("PS", bk)])
                activation(nla[:], ps[bk][:, 0:256], AF.Exp, [("PS", bk)], ["nla"], scale=-1.0)
                activation(nla[:], nla[:], AF.Ln, ["nla"], ["nla"], bias=onec[:, 0:1])
                for c in range(2):
                    b2 = bank()
                    mm_group(ps[b2][:, 0:128], [(nla[:, c * 128:(c + 1) * 128], umask_f[:])], ["nla", "umask_f"], [("PS", b2)])
                    activation(Fv(10 + c)[:, tc * 128:(tc + 1) * 128], ps[b2][:, 0:128], AF.Exp, [("PS", b2)],
                               [("F", 10 + c)], scale=-1.0 / 16)
                    activation(Fv(12 + c)[:, tc * 128:(tc + 1) * 128], ps[b2][:, 0:128], AF.Exp, [("PS", b2)],
                               [("F", 12 + c)], scale=1.0 / 16)
            for c in range(2):
                bk = proj_chunk(f"GQ{c}")
                stt(B(QS[c]), ps[bk][:], 0.125, Fv(10 + c), ALU.mult, ALU.mult, [("PS", bk), ("F", 10 + c)], [("B", QS[c])])
            for c in range(2):
                bk = proj_chunk(f"GK{c}")
                tt(B(KS[c]), ps[bk][:], Fv(12 + c), ALU.mult, [("PS", bk), ("F", 12 + c)], [("B", KS[c])])
            proj_v("GV")
            for c in range(4):
                bk = proj_chunk(f"GG{c}")
                activation(B(GS[c]), ps[bk][:], AF.Silu, [("PS", bk)], [("B", GS[c])])
            run_steps(linattn(1, QS, KS, lambda c, n: (Fv(10 + c)[:, n * 128 + 127:n * 128 + 128], ("F", 10 + c)), GS,
                              [YSL[2] + h for h in range(4)], first))
            if first:
                memset(pbuf[:, :, 0:16], 0.0, [("pbuf", g) for g in range(4)])
            pbks = []
            for g in range(4):
                bk = proj_chunk(f"PU{g}")
                activation(pbuf[:, g, 16:16 + T], ps[bk][:], AF.Copy, [("PS", bk)], [("pbuf", g)])
            wpl, kpl = nextw("POOLW")
            for g in range(4):
                win = (2, 4, 8, 16)[g]
                cur = pbuf[:, g, :]
                k = ("pbuf", g)
                lvl, lk, lo, sh, pi = cur, k, 0, 1, 0
                while sh < win:
                    dsta = Ft[:, 12 + 2 * pi:14 + 2 * pi, :].rearrange("p a b -> p (a b)")
                    dk = ("F", 12 + 2 * pi)
                    dk2 = ("F", 13 + 2 * pi)
                    tt(dsta[:, lo + sh:16 + T], lvl[:, lo + sh:16 + T], lvl[:, lo:16 + T - sh], ALU.add, [lk], [dk, dk2])
                    lvl, lk, lo = dsta, dk, lo + sh
                    sh *= 2
                    pi ^= 1
                stt(Fv(4), lvl[:, 16:16 + T], 1.0 / win, cur[:, 16:16 + T], ALU.mult, ALU.subtract, [lk, k], [("F", 4)])
                if first:
                    tt(Fv(5)[:, 0:16], lvl[:, 16:32], invcnt[:, g, :], ALU.mult, [lk, "invcnt"], [("F", 5)])
                    tt(Fv(4)[:, 0:16], Fv(5)[:, 0:16], cur[:, 16:32], ALU.subtract, [("F", 5), k, ("F", 4)], [("F", 4)])
                copy(B(58), Fv(4), [("F", 4)], [("B", 58)])
                bk = bank()
                mm_group(ps[bk][:], [(lhs(wpl, g), B(58))], [kpl, ("B", 58)], [("PS", bk)])
                ts(B(YSL[3] + g), ps[bk][:], pcol("pool_scale", g), None, ALU.mult, None, [("PS", bk), "par"], [("B", YSL[3] + g)])
                copy(pbuf[:, g, 0:16], pbuf[:, g, T:T + 16], [k], [k], eng="pool")
            SU = (46, 47, 48, 49)
            for c in range(4):
                bk = proj_chunk(f"SU{c}")
                activation(B(SU[c]), ps[bk][:], AF.Copy, [("PS", bk)], [("B", SU[c])])
            s5w = []
            for i3, nm3 in enumerate(DIRECT):
                dst3 = Bt[:, 32 + 4 * i3:36 + 4 * i3, :].rearrange("p a b -> p (a b)")
                k3 = ("B", 32 + 4 * i3)
                dma(dst3, wb[l][PIDX[nm3]], [("wb", l, PIDX[nm3])], [("B", 32 + 4 * i3 + x3) for x3 in range(4)], f"s5w{i3}")
                s5w.append((dst3, k3))
            (wcre, kcre), (wcim, kcim), (wglu, kglu) = s5w
            if first:
                memset(s5z[:], 0.0, ["s5z"])

            def s5_chunk(oc, jl, wcre=wcre, wcim=wcim, kcre=kcre, kcim=kcim):
                bo = 7
                jj = oc * 4 + jl
                par_ = jj % 2
                TC, TN = (14, 15) if par_ == 0 else (10, 11)
                ZR, ZI = (0, 1) if par_ == 0 else (6, 7)
                dma(Fv(TC), rot5_d[jj, 0], [("rot5", jj)], [("F", TC)], f"r5l0{par_}")
                dma(Fv(TN), rot5_d[jj, 1], [("rot5", jj)], [("F", TN)], f"r5l1{par_}")
                COS, NSN = Fv(TC), Fv(TN)
                br_ = bank(); bi_ = bank()
                mm_group(ps[br_][:], [(bbT[:, 0, jj, :], B(SU[oc]))], ["bbT", ("B", SU[oc])], [("PS", br_)])
                mm_group(ps[bi_][:], [(bbT[:, 1, jj, :], B(SU[oc]))], ["bbT", ("B", SU[oc])], [("PS", bi_)])
                tt(Fv(ZR), ps[br_][:], COS, ALU.mult, [("PS", br_), ("F", TC)], [("F", ZR)])
                tt(Fv(12), ps[bi_][:], NSN, ALU.mult, [("PS", bi_), ("F", TN)], [("F", 12)])
                tt(Fv(ZI), ps[bi_][:], COS, ALU.mult, [("PS", bi_), ("F", TC)], [("F", ZI)])
                tt(Fv(13), ps[br_][:], NSN, ALU.mult, [("PS", br_), ("F", TN)], [("F", 13)])
                tt(Fv(ZR), Fv(ZR), Fv(12), ALU.subtract, [("F", ZR), ("F", 12)], [("F", ZR)])
                tt(Fv(ZI), Fv(ZI), Fv(13), ALU.add, [("F", ZI), ("F", 13)], [("F", ZI)])
                rcol = s5m[:, jj, 2:3]
                for (wsl, k2) in ((ZR, 0), (ZI, 1)):
                    dve(lambda e, wsl=wsl, k2=k2: e.tensor_tensor_scan(
                        out=Fv(wsl), data0=rcol.to_broadcast([128, T]), data1=Fv(wsl),
                        initial=s5z[:, jj, k2:k2 + 1], op0=ALU.mult, op1=ALU.add),
                        [("F", wsl), ("s5m", 2), "s5z"], [("F", wsl)])
                    activation(s5zl[:, jj, k2:k2 + 1], Fv(wsl)[:, T - 1:T], AF.Copy, [("F", wsl)], ["s5zl"])
                XR, XI = 50 + 2 * (jl % 2), 51 + 2 * (jl % 2)
                tt(Fv(8), Fv(ZR), COS, ALU.mult, [("F", ZR), ("F", TC)], [("F", 8)], eng="pool")
                tt(Fv(9), Fv(ZI), NSN, ALU.mult, [("F", ZI), ("F", TN)], [("F", 9)], eng="pool")
                tt(B(XR), Fv(8), Fv(9), ALU.add, [("F", 8), ("F", 9)], [("B", XR)], eng="pool")
                tt(Fv(8), Fv(ZR), NSN, ALU.mult, [("F", ZR), ("F", TN)], [("F", 8)], eng="pool")
                tt(Fv(9), Fv(ZI), COS, ALU.mult, [("F", ZI), ("F", TC)], [("F", 9)], eng="pool")
                tt(B(XI), Fv(8), Fv(9), ALU.subtract, [("F", 8), ("F", 9)], [("B", XI)], eng="pool")

            def s5_chunkB(oc, jl, wcre=wcre, wcim=wcim, kcre=kcre, kcim=kcim):
                bo = 7
                jj = oc * 4 + jl
                XR, XI = 50 + 2 * (jl % 2), 51 + 2 * (jl % 2)

                def fn(e):
                    e.matmul(ps[bo][:], lhsT=lhs(wcre, jj), rhs=B(XR), start=(jl == 0), stop=False)
                    return e.matmul(ps[bo][:], lhsT=lhs(wcim, jj), rhs=B(XI), start=False, stop=False)
                pe(fn, [kcre, kcim, ("B", XR), ("B", XI)], [("PS", bo)])

            def s5_epi(oc):
                bo = 7
                pe(lambda e: e.matmul(ps[bo][:], lhsT=dgd[:, oc, :], rhs=B(SU[oc]), start=False, stop=True),
                   ["dgd", ("B", SU[oc])], [("PS", bo)])
                activation(Fv(12), ps[bo][:], AF.Copy, [("PS", bo)], [("F", 12)])
                tt(Fv(13), Fv(12), Fv(12), ALU.mult, [("F", 12)], [("F", 13)])
                ts(Fv(13), Fv(13), 0.044715, 1.0, ALU.mult, ALU.add, [("F", 13)], [("F", 13)])
                tt(Fv(13), Fv(13), Fv(12), ALU.mult, [("F", 13), ("F", 12)], [("F", 13)])
                activation(Fv(13), Fv(13), AF.Sigmoid, [("F", 13)], [("F", 13)], scale=2.0 * math.sqrt(2.0 / math.pi))
                tt(B(54 + oc), Fv(12), Fv(13), ALU.mult, [("F", 12), ("F", 13)], [("B", 54 + oc)])

            def s5_fin(wglu=wglu, kglu=kglu):
                zr, zi = s5zl[:, :, 0], s5zl[:, :, 1]
                c5, sn5 = s5m[:, :, 18], s5m[:, :, 17]
                t0, t1, t2, t3 = s5zt[:, :, 0], s5zt[:, :, 1], s5zt[:, :, 2], s5zt[:, :, 3]
                tt(t0, zr, c5, ALU.mult, ["s5zl", ("s5m", 18)], [("s5zt", 0)])
                tt(t1, zi, sn5, ALU.mult, ["s5zl", ("s5m", 17)], [("s5zt", 1)])
                tt(t2, zr, sn5, ALU.mult, ["s5zl", ("s5m", 17)], [("s5zt", 2)])
                tt(t3, zi, c5, ALU.mult, ["s5zl", ("s5m", 18)], [("s5zt", 3)])
                tt(s5z[:, :, 0], t0, t1, ALU.subtract, [("s5zt", 0), ("s5zt", 1)], ["s5z"])
                tt(s5z[:, :, 1], t2, t3, ALU.add, [("s5zt", 2), ("s5zt", 3)], ["s5z"])
                for oc in range(4):
                    bk = bank()
                    mm_group(ps[bk][:], [(lhs(wglu, oc * 4 + kc), B(54 + kc)) for kc in range(4)],
                             [kglu] + [("B", 54 + kc) for kc in range(4)], [("PS", bk)])
                    activation(Fv(12), ps[bk][:], AF.Sigmoid, [("PS", bk)], [("F", 12)])
                    tt(B(YSL[1] + oc), Fv(12), B(54 + oc), ALU.mult, [("F", 12), ("B", 54 + oc)], [("B", YSL[1] + oc)])

            s5steps = []
            for g in range(17):
                if g < 16:
                    s5steps.append((s5_chunk, (g // 4, g % 4)))
                if g >= 1:
                    gp = g - 1
                    s5steps.append((s5_chunkB, (gp // 4, gp % 4)))
                    if gp % 4 == 3:
                        s5steps.append((s5_epi, (gp // 4,)))
            s5steps.append((s5_fin, ()))
            def pm_step(dc):
                for gi_, n in enumerate((0, 2, 3)):
                    bk = proj_chunk(f"GATE{n}_{dc}")
                    activation(B(58 + gi_), ps[bk][:], AF.Sigmoid, [("PS", bk)], [("B", 58 + gi_)])
                wbr, kbr = nextw(f"WBRA{dc}")
                for gi_, n in enumerate((0, 2, 3)):
                    bk = bank()
                    mm_group(ps[bk][:], [(lhs(wbr, gi_ * 4 + kc), B(YSL[n] + kc)) for kc in range(4)],
                             [kbr] + [("B", YSL[n] + kc) for kc in range(4)], [("PS", bk)])
                    tt(Fv(2 + gi_), ps[bk][:], B(58 + gi_), ALU.mult, [("PS", bk), ("B", 58 + gi_)], [("F", 2 + gi_)])
                tt(Fv(2), Fv(2), Fv(3), ALU.add, [("F", 2), ("F", 3)], [("F", 2)])
                tt(B(MRG + dc), Fv(2), Fv(4), ALU.add, [("F", 2), ("F", 4)], [("B", MRG + dc)])

            run_steps(s5steps, [(pm_step, (dc,)) for dc in range(DC)])
            for q in range(8):
                wbr, kbr = nextw(f"WBRB{q}")
                for dl in range(2):
                    dc = 2 * q + dl
                    bk = proj_chunk(f"GATE1_{dc}")
                    activation(B(61), ps[bk][:], AF.Sigmoid, [("PS", bk)], [("B", 61)])
                    bk = bank()
                    mm_group(ps[bk][:], [(lhs(wbr, dl * 4 + kc), B(YSL[1] + kc)) for kc in range(4)],
                             [kbr] + [("B", YSL[1] + kc) for kc in range(4)], [("PS", bk)])
                    tt(Fv(2 + dl), ps[bk][:], B(61), ALU.mult, [("PS", bk), ("B", 61)], [("F", 2 + dl)])
                    tt(B(MRG + dc), B(MRG + dc), Fv(2 + dl), ALU.add, [("B", MRG + dc), ("F", 2 + dl)], [("B", MRG + dc)])
            for dc in range(DC):
                w, wk = nextw(f"WOUT{dc}")
                bk = bank()
                mm_group(ps[bk][:], [(lhs(w, kc), B(MRG + kc)) for kc in range(DC)],
                         [wk] + [("B", MRG + kc) for kc in range(DC)], [("PS", bk)])
                if dc > 0:
                    fused_mm(dc - 1)
                tt(hT[:, dc, :], hT[:, dc, :], ps[bk][:], ALU.add, [("hT", dc), ("PS", bk)], [("hT", dc)])
                fused_sq(dc)
            fused_mm(DC - 1)
            if first:
                memset(halo[:], 0.0, [("halo", ch) for ch in range(86)], eng="pool")
            rs, rsk = fused_rs()
            for dc in range(DC):
                stt(hn[:, dc, :], hT[:, dc, :], pcol("g_ffn", dc), rs, ALU.mult, ALU.mult,
                    [("hT", dc), "par", rsk], [("hn", dc)])
            for fc in range(FC):
                accs = []
                for k2, (nm, ch) in enumerate(((f"UPA{fc}", fc), (f"UPV{fc}", FC + fc))):
                    bk = proj_chunk(nm, split=(fc == 0 and k2 == 0))
                    ui = k2
                    ub = ubuf[:, ui, :]
                    uk = ("ubuf", ui)
                    copy(ub[:, 0:2], halo[:, ch, :], [("halo", ch)], [uk], eng="pool")
                    activation(ub[:, 2:2 + T], ps[bk][:], AF.Copy, [("PS", bk)], [uk])
                    copy(halo[:, ch, :], ub[:, T:T + 2], [uk], [("halo", ch)], eng="pool")
                    acc = Fv(2 + k2)
                    ak = ("F", 2 + k2)
                    ts(acc, ub[:, 2:2 + T], pcol("cw2", ch), pcol("cb", ch), ALU.mult, ALU.add, [uk, "par"], [ak])
                    stt(acc, ub[:, 1:1 + T], pcol("cw1", ch), acc, ALU.mult, ALU.add, [uk, "par", ak], [ak])
                    stt(acc, ub[:, 0:T], pcol("cw0", ch), acc, ALU.mult, ALU.add, [uk, "par", ak], [ak])
                activation(Fv(4), Fv(2), AF.Silu, [("F", 2)], [("F", 4)])
                tt(B(fc), Fv(4), Fv(3), ALU.mult, [("F", 4), ("F", 3)], [("B", fc)])
            for dc in range(DC):
                ws = [nextw(f"WDN{dc}_{q}") for q in range(3)]
                bk = bank()
                mm_group(ps[bk][:], [(lhs(ws[kc // 16][0], kc % 16), B(kc)) for kc in range(FC)],
                         [w[1] for w in ws] + [("B", kc) for kc in range(FC)], [("PS", bk)])
                if dc > 0:
                    fused_mm(dc - 1)
                tt(hT[:, dc, :], hT[:, dc, :], ps[bk][:], ALU.add, [("hT", dc), ("PS", bk)], [("hT", dc)])
                fused_sq(dc)
            fused_mm(DC - 1)
            rs, rsk = fused_rs()
            if l < depth - 1:
                dma(hscr.rearrange("(c p) s -> p c s", p=128)[:, :, tsl], hT[:],
                    [("hT", dc) for dc in range(DC)], [("hscr", t)], "hst")
                dma(rs_d[t], rs, [rsk], [("rsd", t)], "rsst")
            else:
                for dc in range(DC):
                    stt(hT[:, dc, :], hT[:, dc, :], fing[:, dc:dc + 1], rs, ALU.mult, ALU.mult,
                        [("hT", dc), "fing", rsk], [("hT", dc)])
                dma(outT.rearrange("(c p) s -> p c s", p=128)[:, :, tsl], hT[:],
                    [("hT", dc) for dc in range(DC)], [("out", t)], "ost")
    P.op("sp", lambda e: e.nop(), [("out", t) for t in range(NT)], [])

    P.finalize()
    sems = {}
    for st in P.streams:
        nm = "s_" + "_".join(str(x) for x in st)
        sems[st] = es.enter_context(nc.semaphore(nm))
    with nc.Block() as block:
        @block.tensor
        def _(e):
            P.emit("pe", e, sems)

        @block.scalar
        def _(e):
            P.emit("act", e, sems)

        @block.vector
        def _(e):
            P.emit("dve", e, sems)

        @block.gpsimd
        def _(e):
            P.emit("pool", e, sems)

        @block.sync
        def _(e):
            P.emit("sp", e, sems)
    es.close()
    return nc, cst


def make_in_maps(inp, S, depth, nb):
    cst = build_consts()
    shared = {}
    for l in range(depth):
        shared[f"wf{l}"] = build_layer_pieces(inp, l)
        shared[f"par{l}"] = build_layer_params(inp, l)
        shared[f"bt{l}"] = build_bt(inp, l)
    shared["fing"] = np.ascontiguousarray(inp["final_g"].reshape(16, 128).T)
    for k, v in cst.items():
        shared["c_" + k] = np.ascontiguousarray(v)
    maps = []
    for b in range(nb):
        m = dict(shared)
        m["xT"] = np.ascontiguousarray(inp["x"][b, :S].T)
        m["pos"] = np.ascontiguousarray(inp["positions"][b, :S][None, :].astype(np.int32))
        maps.append(m)
    return maps


def kernel(**inputs):
    inp = {k: np.asarray(v) for k, v in inputs.items()}
    Bn, S, _ = inp["x"].shape
    depth = inp["w_in"].shape[0]
    nc, _ = build_program(S, depth)
    maps = make_in_maps(inp, S, depth, Bn)
    res = run_bass_kernel_spmd(nc, maps, core_ids=list(range(Bn)))
    out = np.stack([np.ascontiguousarray(r["outT"].T) for r in res.results], axis=0)
    return out.astype(np.float32)
```
